# Optimizing a Trainium2 kernel written in Bass

```python
import math
import jax, jax.numpy as jnp
from jax import lax
import numpy as np

D_MODEL = 2048
BATCH = 2
SEQ = 4096
DEPTH = 1

CHUNK = 64
Q_BLOCK = 128
D_DIFF = D_MODEL // 2
D_MLSTM = D_MODEL - D_DIFF
DIFF_HEAD_DIM = 64
N_DIFF_HEADS = D_DIFF // (2 * DIFF_HEAD_DIM)
DIFF_V_DIM = 2 * DIFF_HEAD_DIM
MLSTM_HEAD_DIM = 128
N_MLSTM_HEADS = D_MLSTM // MLSTM_HEAD_DIM
CONV_WIDTH = 4
N_BUCKETS = 32
MAX_DISTANCE = 128
N_GROUPS = 4
EXPERTS_PER_GROUP = 8
TOP_K_INNER = 2
D_EXPERT = D_MODEL // 4
EPS = 1e-6
PROJ_SIZES = (D_DIFF, D_DIFF, D_DIFF,
              D_MLSTM, D_MLSTM, D_MLSTM, D_MLSTM,
              N_MLSTM_HEADS, N_MLSTM_HEADS)
PROJ_DIM = sum(PROJ_SIZES)
PROJ_SPLITS = tuple(int(v) for v in np.cumsum(PROJ_SIZES)[:-1])

kernel_name = "hymba_diffattn_mlstm_hmoe"


def rmsnorm(x, g):
    xf = x.astype(jnp.float32)
    y = xf * lax.rsqrt(jnp.mean(xf * xf, axis=-1, keepdims=True) + EPS)
    return (y * g.astype(jnp.float32)).astype(x.dtype)


def head_rmsnorm(x, g):
    H, dh = x.shape[-2], x.shape[-1]
    return rmsnorm(x, g.reshape(H, dh))


def t5_bucket(rel):
    half = N_BUCKETS // 2
    max_exact = half // 2
    ret = jnp.where(rel > 0, half, 0)
    n = jnp.abs(rel)
    nf = jnp.maximum(n, 1).astype(jnp.float32)
    large = max_exact + (jnp.log(nf / max_exact) / math.log(MAX_DISTANCE / max_exact)
                         * (half - max_exact)).astype(jnp.int32)
    large = jnp.minimum(large, half - 1)
    return ret + jnp.where(n < max_exact, n, large)


def causal_conv(u, w, b):
    C = u.shape[-1]
    y = lax.conv_general_dilated(u, w[:, None, :].astype(u.dtype), window_strides=(1,),
                                 padding=((CONV_WIDTH - 1, 0),),
                                 dimension_numbers=('NWC', 'WIO', 'NWC'),
                                 feature_group_count=C)
    return y + b.astype(u.dtype)


def diff_attention(q1, q2, k1, k2, v, lam, rel_bias):
    B, H, S, dk = q1.shape
    nqb = S // Q_BLOCK
    scale = dk ** -0.5
    key_pos = jnp.arange(S, dtype=jnp.int32)

    def block(args):
        qb1, qb2, qi = args
        q_pos = qi * Q_BLOCK + jnp.arange(Q_BLOCK, dtype=jnp.int32)
        bias = rel_bias[t5_bucket(key_pos[None, :] - q_pos[:, None])]
        bias = jnp.transpose(bias, (2, 0, 1)).astype(jnp.float32)
        mask = (key_pos[None, :] // CHUNK) <= (q_pos[:, None] // CHUNK)

        def probs(qb, k):
            s = jnp.einsum('bhqd,bhkd->bhqk', qb, k).astype(jnp.float32) * scale + bias
            return jax.nn.softmax(jnp.where(mask, s, -jnp.inf), axis=-1)

        p = probs(qb1, k1) - lam * probs(qb2, k2)
        return jnp.einsum('bhqk,bhkd->bhqd', p.astype(v.dtype), v)

    def to_blocks(q):
        return jnp.transpose(q.reshape(B, H, nqb, Q_BLOCK, dk), (2, 0, 1, 3, 4))

    out = lax.map(block, (to_blocks(q1), to_blocks(q2), jnp.arange(nqb, dtype=jnp.int32)))
    return jnp.transpose(out, (1, 2, 0, 3, 4)).reshape(B, H, S, v.shape[-1])


def mlstm_chunkwise(q, k, v, i_pre, f_pre):
    B, H, S, dh = q.shape
    nc, L = S // CHUNK, CHUNK
    q = q.reshape(B, H, nc, L, dh)
    k = k.reshape(B, H, nc, L, dh)
    v = v.reshape(B, H, nc, L, dh)
    log_i = i_pre.reshape(B, H, nc, L)
    log_f = jax.nn.log_sigmoid(f_pre).reshape(B, H, nc, L)
    b = jnp.cumsum(log_f, axis=-1)
    g = b[..., -1]
    w = g[..., None] - b + log_i

    def step(carry, xs):
        C, n, m = carry
        k_c, v_c, g_c, w_c = xs
        m_new = jnp.maximum(g_c + m, jnp.max(w_c, axis=-1))
        decay = jnp.exp(g_c + m - m_new)
        wt = jnp.exp(w_c - m_new[..., None])
        C_new = decay[..., None, None] * C + jnp.einsum('bhl,bhld,bhle->bhde', wt, v_c, k_c)
        n_new = decay[..., None] * n + jnp.einsum('bhl,bhle->bhe', wt, k_c)
        return (C_new, n_new, m_new), (C, n, m)

    init = (jnp.zeros((B, H, dh, dh), jnp.float32), jnp.zeros((B, H, dh), jnp.float32),
            jnp.zeros((B, H), jnp.float32))
    xs = (jnp.moveaxis(k, 2, 0), jnp.moveaxis(v, 2, 0), jnp.moveaxis(g, 2, 0), jnp.moveaxis(w, 2, 0))
    _, (C_prev, n_prev, m_prev) = lax.scan(step, init, xs)
    C_prev = jnp.moveaxis(C_prev, 0, 2)
    n_prev = jnp.moveaxis(n_prev, 0, 2)
    m_prev = jnp.moveaxis(m_prev, 0, 2)

    causal = jnp.tril(jnp.ones((L, L), dtype=bool))
    D = jnp.where(causal, b[..., :, None] - b[..., None, :] + log_i[..., None, :], -jnp.inf)
    m_inter = b + m_prev[..., None]
    m_t = jnp.maximum(jnp.max(D, axis=-1), m_inter)
    Wts = jnp.exp(D - m_t[..., None])
    inter = jnp.exp(m_inter - m_t)
    s_qk = jnp.einsum('bhctd,bhcsd->bhcts', q, k) * Wts
    num = (jnp.einsum('bhcts,bhcsd->bhctd', s_qk, v)
           + inter[..., None] * jnp.einsum('bhcde,bhcte->bhctd', C_prev, q))
    den = jnp.sum(s_qk, axis=-1) + inter * jnp.einsum('bhce,bhcte->bhct', n_prev, q)
    h = num / jnp.maximum(jnp.abs(den), jnp.exp(-m_t))[..., None]
    return h.reshape(B, H, S, dh)


def hier_moe(x, w_group, b_group, w_router, b_router, w_gate, w_up, w_down):
    gl = (x @ w_group).astype(jnp.float32) + b_group.astype(jnp.float32)
    gp = jax.nn.softmax(gl, axis=-1)
    _, gsel = lax.top_k(gl, 1)
    gmask = jax.nn.one_hot(gsel[:, 0], N_GROUPS, dtype=jnp.float32)
    gw = jnp.sum(gp * gmask, axis=-1)
    el = (jnp.einsum('td,gde->tge', x, w_router).astype(jnp.float32)
          + b_router.astype(jnp.float32))
    el_sel = jnp.einsum('tge,tg->te', el, gmask)
    top_v, top_i = lax.top_k(el_sel, TOP_K_INNER)
    top_w = jax.nn.softmax(top_v, axis=-1)
    ew = jnp.sum(jax.nn.one_hot(top_i, EXPERTS_PER_GROUP, dtype=jnp.float32) * top_w[..., None], axis=1)
    comb = (gw[:, None, None] * gmask[:, :, None] * ew[:, None, :]).astype(x.dtype)
    out = jnp.zeros_like(x)
    for gi in range(N_GROUPS):
        hid = (jax.nn.silu(jnp.einsum('td,edf->tef', x, w_gate[gi]))
               * jnp.einsum('td,edf->tef', x, w_up[gi]) * comb[:, gi, :, None])
        out = out + jnp.einsum('tef,efd->td', hid, w_down[gi])
    return out


def setup_inputs(seed: int = 0) -> dict:
    key = jax.random.key(seed)
    ks = jax.random.split(key, 24)
    f32 = jnp.float32

    def nrm(k, shape, scale):
        return jax.random.normal(k, shape, f32) * scale

    L, D = DEPTH, D_MODEL
    G, E, F = N_GROUPS, EXPERTS_PER_GROUP, D_EXPERT
    return {
        "x": nrm(ks[0], (BATCH, SEQ, D), 1.0),
        "rel_bias": nrm(ks[1], (N_BUCKETS, N_DIFF_HEADS), 0.1),
        "ln_mix_g": 1.0 + nrm(ks[2], (L, D), 0.02),
        "w_in": nrm(ks[3], (L, D, PROJ_DIM), D ** -0.5),
        "conv_w": nrm(ks[4], (L, CONV_WIDTH, 2 * D_MLSTM), CONV_WIDTH ** -0.5),
        "conv_b": nrm(ks[5], (L, 2 * D_MLSTM), 0.02),
        "b_i": nrm(ks[6], (L, N_MLSTM_HEADS), 0.1),
        "b_f": 3.0 + nrm(ks[7], (L, N_MLSTM_HEADS), 0.1),
        "lam_q1": nrm(ks[8], (L, DIFF_HEAD_DIM), 0.1),
        "lam_k1": nrm(ks[9], (L, DIFF_HEAD_DIM), 0.1),
        "lam_q2": nrm(ks[10], (L, DIFF_HEAD_DIM), 0.1),
        "lam_k2": nrm(ks[11], (L, DIFF_HEAD_DIM), 0.1),
        "diff_norm_g": 1.0 + nrm(ks[12], (L, D_DIFF), 0.02),
        "mlstm_norm_g": 1.0 + nrm(ks[13], (L, D_MLSTM), 0.02),
        "w_out": nrm(ks[14], (L, D_DIFF + D_MLSTM, D), (D_DIFF + D_MLSTM) ** -0.5),
        "ln_ffn_g": 1.0 + nrm(ks[15], (L, D), 0.02),
        "w_group": nrm(ks[16], (L, D, G), D ** -0.5),
        "b_group": nrm(ks[17], (L, G), 0.01),
        "w_router": nrm(ks[18], (L, G, D, E), D ** -0.5),
        "b_router": nrm(ks[19], (L, G, E), 0.01),
        "w_gate": nrm(ks[20], (L, G, E, D, F), D ** -0.5),
        "w_up": nrm(ks[21], (L, G, E, D, F), D ** -0.5),
        "w_down": nrm(ks[22], (L, G, E, F, D), F ** -0.5),
        "ln_f_g": 1.0 + nrm(ks[23], (D,), 0.02),
    }


def reference(x, rel_bias, ln_mix_g, w_in, conv_w, conv_b, b_i, b_f, lam_q1, lam_k1, lam_q2,
              lam_k2, diff_norm_g, mlstm_norm_g, w_out, ln_ffn_g, w_group, b_group, w_router,
              b_router, w_gate, w_up, w_down, ln_f_g):
    B, S, D = x.shape
    f32 = jnp.float32
    for l in range(DEPTH):
        lambda_init = 0.8 - 0.6 * math.exp(-0.3 * l)
        h = rmsnorm(x, ln_mix_g[l])
        proj = h @ w_in[l]
        q_d, k_d, v_d, q_m, k_m, v_m, o_m, i_m, f_m = jnp.split(proj, PROJ_SPLITS, axis=-1)

        q_d = jnp.transpose(q_d.reshape(B, S, N_DIFF_HEADS, 2, DIFF_HEAD_DIM), (0, 2, 3, 1, 4))
        k_d = jnp.transpose(k_d.reshape(B, S, N_DIFF_HEADS, 2, DIFF_HEAD_DIM), (0, 2, 3, 1, 4))
        v_d = jnp.transpose(v_d.reshape(B, S, N_DIFF_HEADS, DIFF_V_DIM), (0, 2, 1, 3))
        lam = (jnp.exp(jnp.sum(lam_q1[l].astype(f32) * lam_k1[l].astype(f32)))
               - jnp.exp(jnp.sum(lam_q2[l].astype(f32) * lam_k2[l].astype(f32))) + lambda_init)
        a = diff_attention(q_d[:, :, 0], q_d[:, :, 1], k_d[:, :, 0], k_d[:, :, 1], v_d, lam, rel_bias)
        a = head_rmsnorm(jnp.transpose(a, (0, 2, 1, 3)), diff_norm_g[l]) * (1.0 - lambda_init)
        a = a.reshape(B, S, D_DIFF)

        qk = jax.nn.silu(causal_conv(jnp.concatenate([q_m, k_m], axis=-1), conv_w[l], conv_b[l]))
        q_m, k_m = jnp.split(qk, 2, axis=-1)

        def heads(t):
            return jnp.transpose(t.reshape(B, S, N_MLSTM_HEADS, MLSTM_HEAD_DIM), (0, 2, 1, 3)).astype(f32)

        i_pre = jnp.transpose(i_m.astype(f32) + b_i[l].astype(f32), (0, 2, 1))
        f_pre = jnp.transpose(f_m.astype(f32) + b_f[l].astype(f32), (0, 2, 1))
        hm = mlstm_chunkwise(heads(q_m), heads(k_m) * MLSTM_HEAD_DIM ** -0.5, heads(v_m), i_pre, f_pre)
        hm = head_rmsnorm(jnp.transpose(hm, (0, 2, 1, 3)).astype(x.dtype), mlstm_norm_g[l])
        hm = (hm * jax.nn.sigmoid(o_m).reshape(B, S, N_MLSTM_HEADS, MLSTM_HEAD_DIM)).reshape(B, S, D_MLSTM)

        x = x + jnp.concatenate([a, hm], axis=-1) @ w_out[l]

        h = rmsnorm(x, ln_ffn_g[l]).reshape(B * S, D)
        x = x + hier_moe(h, w_group[l], b_group[l], w_router[l], b_router[l],
                         w_gate[l], w_up[l], w_down[l]).reshape(B, S, D)
    return rmsnorm(x, ln_f_g)
```

```python
import math
from contextlib import ExitStack
import numpy as np
import ml_dtypes
import concourse.bass as bass
import concourse.mybir as mybir
from concourse.bass_utils import run_bass_kernel_spmd

F32 = mybir.dt.float32
BF16 = mybir.dt.bfloat16
ALU = mybir.AluOpType
AF = mybir.ActivationFunctionType
AX = mybir.AxisListType

D = 2048
S = 4096
OWN = 1024
NT = 32
NOT_ = 8
T0 = 24
EPS = 1e-6
LAMBDA_INIT = 0.8 - 0.6 * math.exp(0.0)
DEBUG = False
STAGES = 99

def _bucket_table():
    half, max_exact = 16, 8
    rel = np.arange(-255, 256, dtype=np.int32)
    ret = np.where(rel > 0, half, 0)
    n = np.abs(rel)
    nf = np.maximum(n, 1).astype(np.float32)
    large = max_exact + (np.log(nf / np.float32(max_exact)) / np.float32(math.log(128 / max_exact))
                         * np.float32(half - max_exact)).astype(np.int32)
    large = np.minimum(large, half - 1)
    return ret + np.where(n < max_exact, n, large)


class Prog:
    LIM_C = 30000
    LIM_D = 30000

    def __init__(self, nc):
        self.nc = nc
        self.eng = dict(pe=nc.tensor, dve=nc.vector, act=nc.scalar, pool=nc.gpsimd, sp=nc.sync)
        self.q = {k: [] for k in self.eng}
        self.cur = {}
        self.lastw = {}
        self.readers = {}
        self.waited = {}
        self.pending_barrier = {k: [] for k in self.eng}
        self.nsem = 0
        self.dcount = {}

    NDSLOT = 8

    def _tick(self, stream, inc):
        extra = None
        if stream[0] == 'd':
            n = self.dcount.get(stream, 0)
            self.dcount[stream] = n + 1
            skey = (stream[0], stream[1], n % self.NDSLOT)
            if skey not in self.cur or self.cur[skey][1] + inc > self.LIM_D:
                if skey in self.cur:
                    extra = (self.cur[skey][0], self.cur[skey][1])
                sem = self.nc.alloc_semaphore(f"s{self.nsem}_d_{stream[1]}")
                self.nsem += 1
                self.cur[skey] = [sem, 0]
            c = self.cur[skey]
            if c[1] > 0:
                extra = (c[0], c[1])
            c[1] += inc
            return (c[0], c[1]), extra, skey
        if stream not in self.cur or self.cur[stream][1] + inc > self.LIM_C:
            sem = self.nc.alloc_semaphore(f"s{self.nsem}_{stream[0]}_{stream[1]}")
            self.nsem += 1
            self.cur[stream] = [sem, 0]
        c = self.cur[stream]
        c[1] += inc
        return (c[0], c[1]), extra, stream

    def barrier(self):
        toks = [(c[0], c[1]) for c in self.cur.values() if c[1] > 0]
        for e in self.eng:
            self.pending_barrier[e] = list(toks)

    def op(self, eng, fn, reads=(), writes=(), dma=False):
        deps = {}

        def add(tok):
            sem, val, stream = tok
            if stream == ('c', 'pe') and eng == 'pe' and not dma:
                return
            k = id(sem)
            if k not in deps or deps[k][1] < val:
                deps[k] = (sem, val)
        for k in reads:
            if k in self.lastw:
                add(self.lastw[k])
        for k in writes:
            if k in self.lastw:
                add(self.lastw[k])
            for tok in self.readers.get(k, {}).values():
                add(tok)
        for (sem, val) in self.pending_barrier[eng]:
            k = id(sem)
            if k not in deps or deps[k][1] < val:
                deps[k] = (sem, val)
        self.pending_barrier[eng] = []
        stream = ('d' if dma else 'c', eng)
        (sem, val), extra, stream = self._tick(stream, 16 if dma else 1)
        if extra is not None:
            k = id(extra[0])
            if k not in deps or deps[k][1] < extra[1]:
                deps[k] = extra
        waits = []
        for k, (s, v) in deps.items():
            wk = (eng, k)
            if self.waited.get(wk, 0) >= v:
                continue
            self.waited[wk] = v
            waits.append((s, v))
        inc = 16 if dma else 1

        def emit(e, waits=waits, fn=fn, sem=sem, inc=inc):
            for (s, v) in waits:
                e.wait_ge(s, v)
            fn(e).then_inc(sem, inc)
        self.q[eng].append(emit)
        tok = (sem, val, stream)
        for k in reads:
            self.readers.setdefault(k, {})[stream] = tok
        for k in writes:
            self.lastw[k] = tok
            self.readers[k] = {}
        return tok

    def final_wait(self, eng):
        toks = [(c[0], c[1]) for c in self.cur.values() if c[1] > 0]

        def emit(e, toks=toks):
            for (s, v) in toks:
                e.wait_ge(s, v)
        self.q[eng].append(emit)

    def flush(self):
        nc = self.nc
        q = self.q
        self.q = {k: [] for k in self.eng}
        with nc.Block() as block:
            @block.tensor
            def _(e):
                for f in q['pe']:
                    f(e)

            @block.vector
            def _(e):
                for f in q['dve']:
                    f(e)

            @block.scalar
            def _(e):
                for f in q['act']:
                    f(e)

            @block.gpsimd
            def _(e):
                for f in q['pool']:
                    f(e)

            @block.sync
            def _(e):
                for f in q['sp']:
                    f(e)


def bc_ap(handle, n, offset=0, parts=128):
    return bass.AP(handle, offset, [[0, parts], [1, n]])


def build_program():
    nc = bass.Bass("TRN2", target_bir_lowering=False)
    P = Prog(nc)

    def din(name, shape, dt=F32):
        return nc.dram_tensor(name, list(shape), dt, kind="ExternalInput")

    xs = din("xs", [S, D])
    valid = din("valid", [128, NT])
    w_in = din("w_in", [D, 7184])
    w_out = din("w_out", [D, D])
    w_gate = din("w_gate", [32, D, 512])
    w_up = din("w_up", [32, D, 512])
    w_down = din("w_down", [32, 512, D])
    w_rt = din("w_rt", [D, 36])
    b_rt = din("b_rt", [36])
    ln_mix_g = din("ln_mix_g", [D])
    ln_ffn_g = din("ln_ffn_g", [D])
    ln_f_g = din("ln_f_g", [D])
    conv_wt = din("conv_wt", [128, 64])
    conv_b = din("conv_b", [128, 16])
    b_if = din("b_if", [16])
    lam_v = din("lam_v", [4, 64])
    dng = din("diff_norm_g", [1024])
    mng = din("mlstm_norm_g", [1024])
    relb = din("relb", [33, 8])
    Econst = din("Econst", [33, 2 * 128 * 128])
    ident_bf_d = din("ident_bf", [128, 128], BF16)
    ident_f_d = din("ident_f", [128, 128])
    tri_d = din("tri", [128, 3 * 128])
    cmask_d = din("cmask", [128, 128])
    y = nc.dram_tensor("y", [OWN, D], F32, kind="ExternalOutput")
    biasd = nc.dram_tensor("biasd", [8, 2 * 128 * 128], F32)
    catT_d = nc.dram_tensor("catT_d", [16, 128, OWN], BF16)
    dbg = {}
    if DEBUG:
        dbg["cat"] = nc.dram_tensor("dbg_cat", [16, 128, OWN], BF16, kind="ExternalOutput")
        dbg["x1"] = nc.dram_tensor("dbg_x1", [OWN, D], F32, kind="ExternalOutput")
        dbg["gates"] = nc.dram_tensor("dbg_gates", [128, 5 * 256], F32, kind="ExternalOutput")
        dbg["hT"] = nc.dram_tensor("dbg_hT", [128, 16 * 256], BF16, kind="ExternalOutput")
        dbg["bias"] = nc.dram_tensor("dbg_bias", [8, 2 * 128 * 128], F32, kind="ExternalOutput")
        dbg["o12"] = nc.dram_tensor("dbg_o12", [128, 2 * 129], F32, kind="ExternalOutput")
        dbg["pT"] = nc.dram_tensor("dbg_pT", [128, 512], BF16, kind="ExternalOutput")

    def sb(es, name, shape, dt):
        return es.enter_context(nc.sbuf_tensor("sb_" + name, list(shape), dt))

    def ps(es, name, shape, dt=F32):
        return es.enter_context(nc.psum_tensor(name, list(shape), dt))

    with ExitStack() as top:
        banks = [ps(top, f"bank{i}", [128, 512]) for i in range(7)]
        pbf = ps(top, "pbf", [128, 1024], BF16)
        ident_bf = sb(top, "ident_bf_s", [128, 128], BF16)
        ident_f = sb(top, "ident_f_s", [128, 128], F32)
        P.op('sp', lambda e: e.dma_start(out=ident_bf[:, :], in_=ident_bf_d[:, :]), writes=["ident_bf"], dma=True)
        P.op('sp', lambda e: e.dma_start(out=ident_f[:, :], in_=ident_f_d[:, :]), writes=["ident_f"], dma=True)

        eps_t = sb(top, "eps_t", [128, 1], F32)
        P.op('dve', lambda e: e.memset(eps_t[:, :], EPS), writes=["eps_t"])
        bank_rr = [0]

        def next_bank():
            b = bank_rr[0] % 7
            bank_rr[0] += 1
            return b

        with ExitStack() as mx:
            hT = sb(mx, "hT", [128, 16, S], BF16)
            alpha = sb(mx, "alpha", [128, 256], F32)
            alphac = sb(mx, "alphac", [128, 256], F32)
            beta = sb(mx, "beta", [128, 256], F32)
            egA = sb(mx, "egA", [128, 256], F32)
            egB = sb(mx, "egB", [128, 256], F32)
            valid_s = sb(mx, "valid_s", [128, NT], F32)
            P.op('sp', lambda e: e.dma_start(out=valid_s[:, :], in_=valid[:, :]), writes=["valid"], dma=True)

            with ExitStack() as s1:
                g_bc = sb(s1, "g_bc", [128, D], F32)
                P.op('sp', lambda e: e.dma_start(out=g_bc[:, :], in_=bc_ap(ln_mix_g, D)), writes=["g_bc"], dma=True)
                xb = [sb(s1, f"xb{i}", [128, D], F32) for i in range(2)]
                xn = [sb(s1, f"xn{i}", [128, D], BF16) for i in range(2)]
                junk = sb(s1, "junk1", [128, D], BF16)
                ss = sb(s1, "ss1", [128, NT], F32)
                rstd = sb(s1, "rstd1", [128, NT], F32)
                relb_s = sb(s1, "relb_s", [33, 8], F32)
                P.op('pool', lambda e: e.dma_start(out=relb_s[:, :], in_=relb[:, :]), writes=["relb_s"], dma=True)
                Es = [sb(s1, f"Es{i}", [33, 2048], F32) for i in range(2)]
                bstage = [sb(s1, f"bstage{i}", [8, 2048], F32) for i in range(2)]

                def bias_piece(pc):
                    u = pc % 2
                    P.op('pool', lambda e: e.dma_start(out=Es[u][:, :], in_=Econst[:, pc * 2048:(pc + 1) * 2048]),
                         writes=[f"Es{u}"], dma=True)
                    for c in range(4):
                        bk = c % 4
                        P.op('pe', lambda e, c=c, bk=bk: e.matmul(banks[bk][0:8, :], lhsT=relb_s[:, :], rhs=Es[u][:, c * 512:(c + 1) * 512],
                                                                  start=True, stop=True),
                             reads=["relb_s", f"Es{u}"], writes=[f"bank{bk}"])
                        P.op('dve', lambda e, c=c, bk=bk: e.tensor_copy(out=bstage[u][:, c * 512:(c + 1) * 512], in_=banks[bk][0:8, :]),
                             reads=[f"bank{bk}"], writes=[f"bstage{u}"])
                    P.op('pool', lambda e: e.dma_start(out=biasd[:, pc * 2048:(pc + 1) * 2048], in_=bstage[u][:, :]),
                         reads=[f"bstage{u}"], writes=["biasd"], dma=True)

                w_if = sb(s1, "w_if", [128, 16, 16], BF16)
                P.op('pool', lambda e: e.dma_start(out=w_if[:, :, :], in_=w_in[:, 7168:7184].rearrange("(k p) c -> p k c", p=128)),
                     writes=["w_if"], dma=True)
                gbk = banks[4]

                def gate_mm(t):
                    for kc in range(16):
                        P.op('pe', lambda e, t=t, kc=kc: e.matmul(gbk[:, t * 16:(t + 1) * 16], lhsT=hT[:, kc, t * 128:(t + 1) * 128],
                                                                  rhs=w_if[:, kc, :], start=(kc == 0), stop=(kc == 15)),
                             reads=[f"hT{t}", "w_if"], writes=["bank4"])

                for t in range(NT):
                    b = t % 2
                    if t % 2 == 1:
                        bias_piece(t // 2)
                    if t >= 1:
                        gate_mm(t - 1)
                    P.op('sp', lambda e, t=t, b=b: e.dma_start(out=xb[b][:, :], in_=xs[t * 128:(t + 1) * 128, :]),
                         writes=[f"xb{b}"], dma=True)
                    P.op('act', lambda e, t=t, b=b: e.activation(out=junk[:, :], in_=xb[b][:, :], func=AF.Square,
                                                                 accum_out=ss[:, t:t + 1]),
                         reads=[f"xb{b}"], writes=["junk1", f"ss{t}"])
                    P.op('act', lambda e, t=t: e.activation(out=rstd[:, t:t + 1], in_=ss[:, t:t + 1], func=AF.Sqrt, scale=1.0 / D, bias=eps_t[:, 0:1]),
                         reads=[f"ss{t}", "eps_t"], writes=[f"rstd{t}"])
                    P.op('dve', lambda e, t=t: e.reciprocal(out=rstd[:, t:t + 1], in_=rstd[:, t:t + 1]),
                         reads=[f"rstd{t}"], writes=[f"rstd{t}"])
                    P.op('dve', lambda e, t=t, b=b: e.scalar_tensor_tensor(out=xn[b][:, :], in0=xb[b][:, :],
                                                                           scalar=rstd[:, t:t + 1], in1=g_bc[:, :],
                                                                           op0=ALU.mult, op1=ALU.mult),
                         reads=[f"xb{b}", f"rstd{t}", "g_bc"], writes=[f"xn{b}"])
                    for half in range(2):
                        for kk in range(8):
                            kc = half * 8 + kk
                            P.op('pe', lambda e, kc=kc, kk=kk, b=b: e.transpose(out=pbf[:, kk * 128:(kk + 1) * 128],
                                                                                in_=xn[b][:, kc * 128:(kc + 1) * 128],
                                                                                identity=ident_bf[:, :]),
                                 reads=[f"xn{b}", "ident_bf"], writes=["pbf"])
                        eng = 'act' if half == 0 else 'dve'
                        if eng == 'act':
                            P.op('act', lambda e, t=t, half=half: e.copy(out=hT[:, half * 8:(half + 1) * 8, t * 128:(t + 1) * 128],
                                                                          in_=pbf[:, :].rearrange("p (k t) -> p k t", k=8)),
                                 reads=["pbf"], writes=[f"hT{t}"])
                        else:
                            P.op('dve', lambda e, t=t, half=half: e.tensor_copy(out=hT[:, half * 8:(half + 1) * 8, t * 128:(t + 1) * 128],
                                                                                 in_=pbf[:, :].rearrange("p (k t) -> p k t", k=8)),
                                 reads=["pbf"], writes=[f"hT{t}"])
                gate_mm(NT - 1)
                P.barrier()
                P.flush()
            if DEBUG:
                P.op('sp', lambda e: e.dma_start(out=dbg["hT"][:, :].rearrange("p (k t) -> p k t", k=16), in_=hT[:, :, 3072:3072 + 256]),
                     reads=[f"hT{t}" for t in range(NT)], dma=True)

            with ExitStack() as s2:
                gb = sb(s2, "gb", [128, NT, 16], F32)
                P.op('sp', lambda e: e.dma_start(out=gb[:, :, :], in_=bass.AP(b_if, 0, [[0, 128], [0, NT], [1, 16]])),
                     writes=["gb"], dma=True)
                tri = sb(s2, "tri", [128, 3, 128], F32)
                P.op('sp', lambda e: e.dma_start(out=tri[:, :, :], in_=tri_d[:, :].rearrange("p (a l) -> p a l", a=3)),
                     writes=["tri"], dma=True)
                gpre = sb(s2, "gpre", [128, NT, 16], F32)
                ipre = sb(s2, "ipre", [128, NT, 8], F32)
                spv = sb(s2, "spv", [128, NT, 8], F32)
                tmp = sb(s2, "tmpg", [128, 256], F32)
                gbk = banks[4]
                P.op('dve', lambda e: e.tensor_tensor(out=gpre[:, :, :], in0=gbk[:, :].rearrange("p (t c) -> p t c", c=16),
                                                      in1=gb[:, :, :], op=ALU.add),
                     reads=["bank4", "gb"], writes=["gpre"])
                P.op('dve', lambda e: e.tensor_copy(out=ipre[:, :, :], in_=gpre[:, :, 0:8]), reads=["gpre"], writes=["ipre"])
                P.op('act', lambda e: e.activation(out=spv[:, :, :], in_=gpre[:, :, 8:16], func=AF.Exp, scale=-1.0),
                     reads=["gpre"], writes=["spv"])
                P.op('act', lambda e: e.activation(out=spv[:, :, :], in_=spv[:, :, :], func=AF.Ln, bias=1.0, scale=1.0),
                     reads=["spv"], writes=["spv"])
                spf = spv[:, :, :].rearrange("p t c -> p (t c)")
                for a_i, bk in ((0, 1), (1, 2), (2, 3)):
                    P.op('pe', lambda e, a_i=a_i, bk=bk: e.matmul(banks[bk][:, 0:256], lhsT=tri[:, a_i, :], rhs=spf,
                                                                  start=True, stop=True),
                         reads=["tri", "spv"], writes=[f"bank{bk}"])
                P.op('act', lambda e: e.activation(out=alpha[:, :], in_=banks[1][:, 0:256], func=AF.Exp, scale=-1.0),
                     reads=["bank1"], writes=["alpha"])
                P.op('dve', lambda e: e.tensor_scalar(out=alphac[:, :], in0=alpha[:, :], scalar1=128.0 ** -0.5, scalar2=None,
                                                      op0=ALU.mult),
                     reads=["alpha"], writes=["alphac"])
                P.op('dve', lambda e: e.tensor_tensor(out=tmp[:, :], in0=banks[1][:, 0:256],
                                                      in1=ipre[:, :, :].rearrange("p t c -> p (t c)"), op=ALU.add),
                     reads=["bank1", "ipre", "alpha"], writes=["tmpg"])
                P.op('act', lambda e: e.activation(out=beta[:, :], in_=tmp[:, :], func=AF.Exp), reads=["tmpg"], writes=["beta"])
                P.op('act', lambda e: e.activation(out=egA[:, :], in_=banks[2][:, 0:256], func=AF.Exp, scale=-1.0),
                     reads=["bank2"], writes=["egA"])
                P.op('act', lambda e: e.activation(out=egB[:, :], in_=banks[3][:, 0:256], func=AF.Exp, scale=-1.0),
                     reads=["bank3"], writes=["egB"])
                if DEBUG:
                    for i_, tt in enumerate((alpha, beta, egA, egB, alphac)):
                        P.op('sp', lambda e, i_=i_, tt=tt: e.dma_start(out=dbg["gates"][:, i_ * 256:(i_ + 1) * 256], in_=tt[:, :]),
                             reads=["alpha", "beta", "egA", "egB", "alphac"], dma=True)
                P.barrier()
                P.flush()

            if STAGES >= 3:
                mixer_heads(nc, P, mx, locals())
        if STAGES >= 4:
            ffn_phase(nc, P, top, locals())
        P.final_wait('sp')
        P.final_wait('pool')
        P.flush()
    return nc


def mixer_heads(nc, P, mx, L):
    eps_t = L["eps_t"]
    hT = L["hT"]; banks = L["banks"]; pbf = L["pbf"]; ident_bf = L["ident_bf"]
    alpha = L["alpha"]; alphac = L["alphac"]; beta = L["beta"]; egA = L["egA"]; egB = L["egB"]
    valid_s = L["valid_s"]; w_in = L["w_in"]; sb = L["sb"]; dbg = L["dbg"]
    conv_wt = L["conv_wt"]; conv_b = L["conv_b"]; lam_v = L["lam_v"]; dng = L["dng"]; mng = L["mng"]
    relb = L["relb"]; Econst = L["Econst"]; biasd = L["biasd"]; catT_d = L["catT_d"]; cmask_d = L["cmask_d"]
    es = mx
    cw = sb(es, "cw", [128, 16, 4], F32)
    P.op('sp', lambda e: e.dma_start(out=cw[:, :, :], in_=conv_wt[:, :].rearrange("p (g j) -> p g j", j=4)), writes=["cw"], dma=True)
    cb = sb(es, "cb", [128, 16], F32)
    P.op('sp', lambda e: e.dma_start(out=cb[:, :], in_=conv_b[:, :]), writes=["cb"], dma=True)
    gd_bc = sb(es, "gd_bc", [128, 128], F32)
    gm_bc = sb(es, "gm_bc", [128, 128], F32)
    cmask = sb(es, "cmask", [128, 128], F32)
    P.op('sp', lambda e: e.dma_start(out=cmask[:, :], in_=cmask_d[:, :]), writes=["cmask"], dma=True)
    c15 = sb(es, "c15", [128, 8], F32)
    P.op('sp', lambda e: e.dma_start(out=c15[:, :], in_=bc_ap(relb, 8, offset=15 * 8)), writes=["c15"], dma=True)
    lamt = sb(es, "lamt", [128, 4, 64], F32)
    P.op('sp', lambda e: e.dma_start(out=lamt[:, :, :], in_=bass.AP(lam_v, 0, [[0, 128], [64, 4], [1, 64]])), writes=["lamt"], dma=True)
    lamp = sb(es, "lamp", [128, 2, 64], F32)
    lams = sb(es, "lams", [128, 2], F32)
    nlam = sb(es, "nlam", [128, 1], F32)
    P.op('dve', lambda e: e.tensor_tensor(out=lamp[:, 0, :], in0=lamt[:, 0, :], in1=lamt[:, 1, :], op=ALU.mult), reads=["lamt"], writes=["lamp"])
    P.op('dve', lambda e: e.tensor_tensor(out=lamp[:, 1, :], in0=lamt[:, 2, :], in1=lamt[:, 3, :], op=ALU.mult), reads=["lamt", "lamp"], writes=["lamp"])
    P.op('dve', lambda e: e.tensor_reduce(out=lams[:, :], in_=lamp[:, :, :], axis=AX.X, op=ALU.add), reads=["lamp"], writes=["lams"])
    P.op('act', lambda e: e.activation(out=lams[:, :], in_=lams[:, :], func=AF.Exp), reads=["lams"], writes=["lams"])
    P.op('dve', lambda e: e.tensor_tensor(out=nlam[:, :], in0=lams[:, 1:2], in1=lams[:, 0:1], op=ALU.subtract), reads=["lams"], writes=["nlam"])
    P.op('dve', lambda e: e.tensor_scalar(out=nlam[:, :], in0=nlam[:, :], scalar1=-LAMBDA_INIT, scalar2=None, op0=ALU.add), reads=["nlam"], writes=["nlam"])
    Bnear = sb(es, "Bnear", [128, 2, 128], F32)

    kT_d = sb(es, "kT_d", [128, S], BF16)
    qT_d = sb(es, "qT_d", [128, OWN], BF16)
    v_d = sb(es, "v_d", [128, NT, 129], BF16)
    kT_m = kT_d
    k_tok = sb(es, "k_tok", [128, NT, 128], BF16)
    Vp = v_d
    qT_m = qT_d
    qTA = sb(es, "qTA", [128, OWN], BF16)
    qTB = sb(es, "qTB", [128, OWN], BF16)
    o_sig = sb(es, "o_sig", [128, NOT_, 128], BF16)
    aT_h = sb(es, "aT_h", [128, OWN], BF16)
    hmT_h = aT_h
    ub = [sb(es, f"ub{i}", [128, 516], F32) for i in range(2)]
    uacc = [sb(es, f"uacc{i}", [128, 512], F32) for i in range(2)]
    wb = [sb(es, f"wb{i}", [128, 16, 128], BF16) for i in range(2)]
    pT = [sb(es, f"pT{i}", [128, 512], BF16) for i in range(4)]
    nrt = [sb(es, f"nrt{i}", [128, 256], F32) for i in range(2)]
    Z = sb(es, "Z", [128, 129], F32)
    CTb = [sb(es, f"CTb{i}", [128, 129], BF16) for i in range(16)]
    SM = [sb(es, f"SM{i}", [128, 128], BF16) for i in range(4)]
    smo = sb(es, "smo", [128, NOT_, 4], F32)
    sm1 = [sb(es, f"sm1_{i}", [128, 8], F32) for i in range(2)]
    a1 = [sb(es, f"a1_{i}", [128, 128], F32) for i in range(2)]
    a2 = [sb(es, f"a2_{i}", [128, 128], F32) for i in range(2)]
    abf = [sb(es, f"abf{i}", [128, 128], BF16) for i in range(2)]
    junk = sb(es, "junk3", [128, 128], BF16)
    a2all = sb(es, "a2all", [128, NOT_, 128], F32)
    ssall = sb(es, "ssall", [128, NOT_], F32)
    dbgt = None
    sb_small_qprev = sb(es, "qprev", [128, 4], F32)
    print("SBUF remaining after mixer alloc:", nc.sbuf_bytes_remaining)
    wrr = [0]
    brr = [0]
    srr = [0]

    def nb():
        b = brr[0] % 7
        brr[0] += 1
        return b

    def load_w(c0):
        i = wrr[0] % 2
        wrr[0] += 1
        for hf in range(2):
            P.op('pool', lambda e, i=i, c0=c0, hf=hf: e.dma_start(
                out=wb[i][:, hf * 8:(hf + 1) * 8, :],
                in_=w_in[hf * 1024:(hf + 1) * 1024, c0:c0 + 128].rearrange("(k p) c -> p k c", p=128)),
                writes=[f"wb{i}"], dma=True)
        return i

    def proj_fm(wi, tok0, n, evac):
        bk = nb()
        for kc in range(16):
            P.op('pe', lambda e, kc=kc, bk=bk: e.matmul(banks[bk][:, 0:n], lhsT=wb[wi][:, kc, :], rhs=hT[:, kc, tok0:tok0 + n],
                                                        start=(kc == 0), stop=(kc == 15)),
                 reads=[f"wb{wi}"] + [f"hT{t}" for t in range(tok0 // 128, (tok0 + n) // 128)], writes=[f"bank{bk}"])
        evac(bk)

    def proj_tm(wi, t0, nt, evac):
        bk = nb()
        for j in range(nt):
            t = t0 + j
            for kc in range(16):
                P.op('pe', lambda e, kc=kc, bk=bk, j=j, t=t: e.matmul(banks[bk][:, j * 128:(j + 1) * 128],
                                                                     lhsT=hT[:, kc, t * 128:(t + 1) * 128], rhs=wb[wi][:, kc, :],
                                                                     start=(kc == 0), stop=(kc == 15)),
                     reads=[f"wb{wi}", f"hT{t}"], writes=[f"bank{bk}"])
        evac(bk)

    next_kd = [None]
    for h in range(8):
        P.op('sp', lambda e, h=h: e.dma_start(out=gd_bc[:, :], in_=bc_ap(dng, 128, offset=h * 128)), writes=["gd_bc"], dma=True)
        P.op('sp', lambda e, h=h: e.dma_start(out=gm_bc[:, :], in_=bc_ap(mng, 128, offset=h * 128)), writes=["gm_bc"], dma=True)
        P.op('sp', lambda e, h=h: e.dma_start(out=Bnear[:, :, :], in_=biasd[h, :].rearrange("(d k q) -> k d q", d=2, k=128)),
             reads=["biasd"], writes=["Bnear"], dma=True)
        wi = next_kd[0] if next_kd[0] is not None else load_w(1024 + h * 128)
        for blk in range(8):
            proj_fm(wi, blk * 512, 512, lambda bk, blk=blk: P.op(
                'act', lambda e: e.copy(out=kT_d[:, blk * 512:(blk + 1) * 512], in_=banks[bk][:, :]),
                reads=[f"bank{bk}"], writes=["kT_d"]))
        wi = load_w(h * 128)
        for blk in range(2):
            proj_fm(wi, 3072 + blk * 512, 512, lambda bk, blk=blk: P.op(
                'dve', lambda e: e.tensor_copy(out=qT_d[:, blk * 512:(blk + 1) * 512], in_=banks[bk][:, :]),
                reads=[f"bank{bk}"], writes=["qT_d"]))
        P.op('dve', lambda e: e.tensor_copy(out=v_d[:, :, 128], in_=valid_s[:, :]), reads=["valid"], writes=["v_d"])
        wi = load_w(2048 + h * 128)
        for g4 in range(8):
            proj_tm(wi, g4 * 4, 4, lambda bk, g4=g4: P.op(
                'act', lambda e: e.copy(out=v_d[:, g4 * 4:(g4 + 1) * 4, 0:128], in_=banks[bk][:, :].rearrange("p (j c) -> p j c", j=4)),
                reads=[f"bank{bk}"], writes=["v_d"]))
        items = []
        for i in range(NOT_):
            qt = T0 + i
            far = list(range(0, qt - 1))
            groups = [far[j:j + 4] for j in range(0, len(far), 4)] + [[qt - 1, qt]]
            for gi, grp in enumerate(groups):
                items.append(dict(i=i, grp=grp, near=(gi == len(groups) - 1), first=(gi == 0)))

        def emit_qk(n):
            it = items[n]
            i = it["i"]
            for j, kt in enumerate(it["grp"]):
                for m in range(2):
                    sbk = 2 * (n % 2) + m
                    P.op('pe', lambda e, j=j, kt=kt, sbk=sbk, m=m, i=i: e.matmul(
                        banks[sbk][:, j * 128:(j + 1) * 128], lhsT=kT_d[m * 64:(m + 1) * 64, kt * 128:(kt + 1) * 128],
                        rhs=qT_d[m * 64:(m + 1) * 64, i * 128:(i + 1) * 128], start=True, stop=True),
                        reads=["kT_d", "qT_d"], writes=[f"bank{sbk}"])

        def emit_exp(n):
            it = items[n]
            nn = len(it["grp"]) * 128
            for m in range(2):
                sbk = 2 * (n % 2) + m
                pi = 2 * (n % 2) + m
                if not it["near"]:
                    P.op('act', lambda e, sbk=sbk, nn=nn, pi=pi, h=h: e.activation(out=pT[pi][:, 0:nn], in_=banks[sbk][:, 0:nn], func=AF.Exp,
                                                                                 scale=0.125, bias=c15[:, h:h + 1]),
                         reads=[f"bank{sbk}", "c15"], writes=[f"pT{pi}"])
                else:
                    P.op('dve', lambda e, sbk=sbk, h=h, m=m: e.scalar_tensor_tensor(
                        out=nrt[m][:, :], in0=banks[sbk][:, 0:256], scalar=0.125,
                        in1=Bnear[:, :, :].rearrange("k d q -> k (d q)"), op0=ALU.mult, op1=ALU.add),
                        reads=[f"bank{sbk}", "Bnear"], writes=[f"nrt{m}"])
                    P.op('act', lambda e, pi=pi, m=m: e.activation(out=pT[pi][:, 0:256], in_=nrt[m][:, :], func=AF.Exp),
                         reads=[f"nrt{m}"], writes=[f"pT{pi}"])

        def emit_pv(n):
            it = items[n]
            i = it["i"]
            for m in range(2):
                pi = 2 * (n % 2) + m
                obk = 4 + m
                for j, kt in enumerate(it["grp"]):
                    first = it["first"] and j == 0
                    last = it["near"] and (j == len(it["grp"]) - 1)
                    P.op('pe', lambda e, j=j, kt=kt, pi=pi, obk=obk, first=first, last=last: e.matmul(
                        banks[obk][:, 0:129], lhsT=pT[pi][:, j * 128:(j + 1) * 128], rhs=v_d[:, kt, :], start=first, stop=last),
                        reads=[f"pT{pi}", "v_d"], writes=[f"bank{obk}"])
            if it["near"]:
                combine(i)

        def combine(i):
            u = i % 2
            ob = [4, 5]
            o1, o2 = banks[ob[0]], banks[ob[1]]
            P.op('dve', lambda e, u=u, o1=o1: e.reciprocal(out=sm1[u][:, 0:1], in_=o1[:, 128:129]), reads=[f"bank{ob[0]}"], writes=[f"sm1_{u}"])
            P.op('dve', lambda e, u=u, o2=o2: e.reciprocal(out=sm1[u][:, 1:2], in_=o2[:, 128:129]), reads=[f"bank{ob[1]}", f"sm1_{u}"], writes=[f"sm1_{u}"])
            P.op('dve', lambda e, u=u: e.tensor_tensor(out=sm1[u][:, 1:2], in0=sm1[u][:, 1:2], in1=nlam[:, 0:1], op=ALU.mult),
                 reads=[f"sm1_{u}", "nlam"], writes=[f"sm1_{u}"])
            P.op('dve', lambda e, u=u, o1=o1: e.tensor_scalar(out=a1[u][:, :], in0=o1[:, 0:128], scalar1=sm1[u][:, 0:1], scalar2=None, op0=ALU.mult),
                 reads=[f"bank{ob[0]}", f"sm1_{u}"], writes=[f"a1_{u}"])
            P.op('dve', lambda e, u=u, o2=o2, i=i: e.scalar_tensor_tensor(out=a2all[:, i, :], in0=o2[:, 0:128], scalar=sm1[u][:, 1:2], in1=a1[u][:, :],
                                                                          op0=ALU.mult, op1=ALU.add),
                 reads=[f"bank{ob[1]}", f"sm1_{u}", f"a1_{u}"], writes=[f"a2all{i}"])
            P.op('dve', lambda e, i=i: e.scalar_tensor_tensor(out=junk[:, :], in0=a2all[:, i, :], scalar=1.0, in1=a2all[:, i, :],
                                                              op0=ALU.mult, op1=ALU.mult, accum_out=ssall[:, i:i + 1]),
                 reads=[f"a2all{i}", f"ssall{i}"], writes=["junk3", f"ssall{i}"])

        def finish_attn():
            ssk = [f"ssall{i}" for i in range(NOT_)]
            P.op('act', lambda e: e.activation(out=ssall[:, :], in_=ssall[:, :], func=AF.Sqrt, scale=1.0 / 128, bias=eps_t[:, 0:1]),
                 reads=ssk + ["eps_t"], writes=ssk)
            P.op('dve', lambda e: e.reciprocal(out=ssall[:, :], in_=ssall[:, :]), reads=ssk, writes=ssk)
            P.op('dve', lambda e: e.tensor_scalar(out=ssall[:, :], in0=ssall[:, :], scalar1=(1.0 - LAMBDA_INIT), scalar2=None, op0=ALU.mult),
                 reads=ssk, writes=ssk)

        def finish_attn_T():
            for i in range(NOT_):
                u = i % 2
                P.op('dve', lambda e, i=i, u=u: e.scalar_tensor_tensor(out=abf[u][:, :], in0=a2all[:, i, :], scalar=ssall[:, i:i + 1], in1=gd_bc[:, :],
                                                                       op0=ALU.mult, op1=ALU.mult),
                     reads=[f"a2all{i}", f"ssall{i}", "gd_bc"], writes=[f"abf{u}"])
                P.op('pe', lambda e, i=i, u=u: e.transpose(out=pbf[:, i * 128:(i + 1) * 128], in_=abf[u][:, :], identity=ident_bf[:, :]),
                     reads=[f"abf{u}", "ident_bf"], writes=["pbf"])
            P.op('dve', lambda e: e.tensor_copy(out=aT_h[:, :], in_=pbf[:, :]), reads=["pbf"], writes=["aT_h"])

        LA = 1
        NI = len(items)
        for n in range(min(LA, NI)):
            emit_qk(n)
        for n in range(NI):
            if n + LA < NI:
                emit_qk(n + LA)
            emit_exp(n)
            emit_pv(n)
        finish_attn()

        wi = load_w(4096 + h * 128)
        urr = [0]

        def conv_blk(bk, g, dst, d0, first_zero, prev):
            r = urr[0] % 2
            urr[0] += 1
            P.op('act', lambda e: e.copy(out=ub[r][:, 4:516], in_=banks[bk][:, :]), reads=[f"bank{bk}"], writes=[f"ub{r}"])
            if first_zero:
                P.op('dve', lambda e: e.memset(ub[r][:, 0:4], 0.0), writes=[f"ub{r}"])
            elif prev is None:
                P.op('dve', lambda e: e.tensor_copy(out=ub[r][:, 1:4], in_=ub[1 - r][:, 513:516]), reads=[f"ub{1 - r}"], writes=[f"ub{r}"])
            else:
                prev(r)
            P.op('dve', lambda e: e.tensor_scalar(out=uacc[r][:, :], in0=ub[r][:, 1:513], scalar1=cw[:, g, 0:1], scalar2=cb[:, g:g + 1],
                                                  op0=ALU.mult, op1=ALU.add),
                 reads=[f"ub{r}", "cw", "cb"], writes=[f"uacc{r}"])
            for j in range(1, 4):
                P.op('dve', lambda e, j=j: e.scalar_tensor_tensor(out=uacc[r][:, :], in0=ub[r][:, 1 + j:513 + j], scalar=cw[:, g, j:j + 1],
                                                                  in1=uacc[r][:, :], op0=ALU.mult, op1=ALU.add),
                     reads=[f"ub{r}", "cw", f"uacc{r}"], writes=[f"uacc{r}"])
            P.op('act', lambda e: e.activation(out=dst[:, d0:d0 + 512], in_=uacc[r][:, :], func=AF.Silu), reads=[f"uacc{r}"], writes=["kT_d" if dst is kT_m else "qT_d"])

        for blk in range(8):
            proj_fm(wi, blk * 512, 512, lambda bk, blk=blk: conv_blk(bk, 8 + h, kT_m, blk * 512, blk == 0, None))
            if blk == 1:
                finish_attn_T()
                P.op('sp', lambda e, h=h: e.dma_start(out=catT_d[h, :, :], in_=aT_h[:, :]), reads=["aT_h"], writes=["catT_d"], dma=True)
        for g8 in range(4):
            for j in range(8):
                t = g8 * 8 + j
                P.op('pe', lambda e, j=j, t=t: e.transpose(out=pbf[:, j * 128:(j + 1) * 128], in_=kT_m[:, t * 128:(t + 1) * 128],
                                                           identity=ident_bf[:, :]),
                     reads=["kT_d", "ident_bf"], writes=["pbf"])
            P.op('act', lambda e, g8=g8: e.copy(out=k_tok[:, g8 * 8:(g8 + 1) * 8, :], in_=pbf[:, :].rearrange("p (j c) -> p j c", j=8)),
                 reads=["pbf"], writes=["k_tok"])
        P.op('dve', lambda e, h=h: e.tensor_tensor(out=Vp[:, :, 128], in0=beta[:, :].rearrange("p (t c) -> p t c", c=8)[:, :, h],
                                                   in1=valid_s[:, :], op=ALU.mult),
             reads=["beta", "valid", "v_d"], writes=["v_d"])
        wi = load_w(5120 + h * 128)
        P.op('dve', lambda e: e.memset(Z[:, :], 0.0), writes=["Z"])

        def scan_step(c):
            t, half = c // 2, c % 2
            r0 = half * 64
            if c > 0:
                pc = c - 1
                egp = (egA if pc % 2 == 0 else egB)
                col = (pc // 2) * 8 + h
                egap = egp[:, col:col + 1]
            else:
                egap = egA[:, h:h + 1]
            if c >= 48:
                ci = c - 48
                P.op('act', lambda e, ci=ci, egap=egap: e.activation(out=CTb[ci][:, :], in_=Z[:, :], func=AF.Copy, scale=egap),
                     reads=["Z", "egA", "egB"], writes=[f"CTb{ci}"])
            bk = nb()
            P.op('pe', lambda e, bk=bk, t=t, r0=r0: e.matmul(banks[bk][:, 0:129], lhsT=k_tok[r0:r0 + 64, t, :], rhs=Vp[r0:r0 + 64, t, :],
                                                             start=True, stop=True),
                 reads=["k_tok", "v_d"], writes=[f"bank{bk}"])
            P.op('dve', lambda e, bk=bk, egap=egap: e.scalar_tensor_tensor(out=Z[:, :], in0=Z[:, :], scalar=egap, in1=banks[bk][:, 0:129],
                                                                           op0=ALU.mult, op1=ALU.add),
                 reads=["Z", f"bank{bk}", "egA", "egB"], writes=["Z"])

        for g4 in range(8):
            def ev(bk, g4=g4):
                for j in range(4):
                    t = g4 * 4 + j
                    P.op('dve', lambda e, j=j, t=t, bk=bk: e.tensor_scalar(out=Vp[:, t, 0:128], in0=banks[bk][:, j * 128:(j + 1) * 128],
                                                                           scalar1=beta[:, t * 8 + h:t * 8 + h + 1], scalar2=None, op0=ALU.mult),
                         reads=[f"bank{bk}", "beta"], writes=["v_d"])
            proj_tm(wi, g4 * 4, 4, ev)
            if g4 >= 1:
                for c in range(8 * (g4 - 1), 8 * g4):
                    scan_step(c)
        wi = load_w(6144 + h * 128)
        for g4 in range(2):
            proj_tm(wi, T0 + g4 * 4, 4, lambda bk, g4=g4: P.op(
                'act', lambda e: e.activation(out=o_sig[:, g4 * 4:(g4 + 1) * 4, :], in_=banks[bk][:, :].rearrange("p (j c) -> p j c", j=4),
                                              func=AF.Sigmoid),
                reads=[f"bank{bk}"], writes=["o_sig"]))
        for c in range(56, 64):
            scan_step(c)
        wi = load_w(3072 + h * 128)
        qprev = sb_small_qprev
        proj_fm(wi, 3072 - 128, 128, lambda bk: P.op(
            'act', lambda e: e.copy(out=qprev[:, 0:3], in_=banks[bk][:, 125:128]), reads=[f"bank{bk}"], writes=["qprev"]))
        proj_fm(wi, 3072, 512, lambda bk: conv_blk(bk, h, qT_m, 0, False, lambda r: P.op(
            'dve', lambda e: e.tensor_copy(out=ub[r][:, 1:4], in_=qprev[:, 0:3]), reads=["qprev"], writes=[f"ub{r}"])))
        proj_fm(wi, 3072 + 512, 512, lambda bk: conv_blk(bk, h, qT_m, 512, False, None))
        P.op('dve', lambda e: e.memset(qTA[:, :], 0.0), writes=["qTA"])
        P.op('dve', lambda e: e.memset(qTB[:, :], 0.0), writes=["qTB"])
        P.op('dve', lambda e: e.tensor_copy(out=qTA[:, :].rearrange("p (i two l) -> p i two l", two=2, l=64)[:, :, 0, :],
                                            in_=qT_m[:, :].rearrange("p (i two l) -> p i two l", two=2, l=64)[:, :, 0, :]),
             reads=["qT_d", "qTA"], writes=["qTA"])
        P.op('dve', lambda e: e.tensor_copy(out=qTB[:, :].rearrange("p (i two l) -> p i two l", two=2, l=64)[:, :, 1, :],
                                            in_=qT_m[:, :].rearrange("p (i two l) -> p i two l", two=2, l=64)[:, :, 1, :]),
             reads=["qT_d", "qTB"], writes=["qTB"])
        for hf in range(2):
            tiles = list(range(hf * 4, hf * 4 + 4))
            sbk = nb()
            for jj, i in enumerate(tiles):
                t = T0 + i
                P.op('pe', lambda e, sbk=sbk, t=t, i=i, jj=jj: e.matmul(banks[sbk][:, jj * 128:(jj + 1) * 128], lhsT=kT_m[:, t * 128:(t + 1) * 128],
                                                                        rhs=qT_m[:, i * 128:(i + 1) * 128], start=True, stop=True),
                     reads=["kT_d", "qT_d"], writes=[f"bank{sbk}"])
            for jj, i in enumerate(tiles):
                P.op('dve', lambda e, sbk=sbk, jj=jj: e.tensor_tensor(out=SM[jj][:, :], in0=banks[sbk][:, jj * 128:(jj + 1) * 128], in1=cmask[:, :],
                                                                      op=ALU.mult),
                     reads=[f"bank{sbk}", "cmask"], writes=[f"SM{jj}"])
            nbks = [nb(), nb()]
            views = []
            for jj, i in enumerate(tiles):
                t = T0 + i
                nbk = nbks[jj // 2]
                off = (jj % 2) * 129
                views.append((nbk, off))
                P.op('pe', lambda e, nbk=nbk, off=off, jj=jj, t=t: e.matmul(banks[nbk][:, off:off + 129], lhsT=SM[jj][:, :], rhs=Vp[:, t, :],
                                                                            start=True, stop=False),
                     reads=[f"SM{jj}", "v_d"], writes=[f"bank{nbk}"])
                P.op('pe', lambda e, nbk=nbk, off=off, i=i: e.matmul(banks[nbk][:, off:off + 129], lhsT=qTA[:, i * 128:(i + 1) * 128],
                                                                     rhs=CTb[2 * i][:, :], start=False, stop=False),
                     reads=["qTA", f"CTb{2 * i}"], writes=[f"bank{nbk}"])
                P.op('pe', lambda e, nbk=nbk, off=off, i=i: e.matmul(banks[nbk][:, off:off + 129], lhsT=qTB[:, i * 128:(i + 1) * 128],
                                                                     rhs=CTb[2 * i + 1][:, :], start=False, stop=True),
                     reads=["qTB", f"CTb{2 * i + 1}"], writes=[f"bank{nbk}"])

            def den(jj):
                nbk, off = views[jj]
                return banks[nbk][:, off + 128:off + 129]
            for jj, i in enumerate(tiles):
                P.op('dve', lambda e, jj=jj, i=i: e.tensor_scalar(out=smo[:, i, 3:4], in0=den(jj), scalar1=-1.0, scalar2=None, op0=ALU.mult),
                     reads=[f"bank{views[jj][0]}"], writes=[f"smo{i}"])
            for jj, i in enumerate(tiles):
                P.op('dve', lambda e, jj=jj, i=i: e.tensor_tensor(out=smo[:, i, 2:3], in0=den(jj), in1=smo[:, i, 3:4], op=ALU.max),
                     reads=[f"bank{views[jj][0]}", f"smo{i}"], writes=[f"smo{i}"])
            for jj, i in enumerate(tiles):
                col = (T0 + i) * 8 + h
                P.op('dve', lambda e, i=i, col=col: e.tensor_tensor(out=smo[:, i, 2:3], in0=smo[:, i, 2:3], in1=alphac[:, col:col + 1], op=ALU.mult),
                     reads=[f"smo{i}", "alphac"], writes=[f"smo{i}"])
            for jj, i in enumerate(tiles):
                P.op('dve', lambda e, i=i: e.tensor_scalar(out=smo[:, i, 2:3], in0=smo[:, i, 2:3], scalar1=1.0, scalar2=None, op0=ALU.max),
                     reads=[f"smo{i}"], writes=[f"smo{i}"])
            for jj, i in enumerate(tiles):
                P.op('dve', lambda e, i=i: e.reciprocal(out=smo[:, i, 2:3], in_=smo[:, i, 2:3]), reads=[f"smo{i}"], writes=[f"smo{i}"])
            for jj, i in enumerate(tiles):
                col = (T0 + i) * 8 + h
                P.op('dve', lambda e, i=i, col=col: e.tensor_tensor(out=smo[:, i, 2:3], in0=smo[:, i, 2:3], in1=alphac[:, col:col + 1], op=ALU.mult),
                     reads=[f"smo{i}", "alphac"], writes=[f"smo{i}"])
            for jj, i in enumerate(tiles):
                nbk, off = views[jj]
                P.op('dve', lambda e, nbk=nbk, off=off, i=i: e.tensor_scalar(out=a2all[:, i, :], in0=banks[nbk][:, off:off + 128], scalar1=smo[:, i, 2:3],
                                                                             scalar2=None, op0=ALU.mult),
                     reads=[f"bank{nbk}", f"smo{i}"], writes=[f"a2all{i}"])
            for jj, i in enumerate(tiles):
                P.op('dve', lambda e, i=i: e.scalar_tensor_tensor(out=junk[:, :], in0=a2all[:, i, :], scalar=1.0, in1=a2all[:, i, :],
                                                                  op0=ALU.mult, op1=ALU.mult, accum_out=ssall[:, i:i + 1]),
                     reads=[f"a2all{i}", f"ssall{i}"], writes=["junk3", f"ssall{i}"])
        ssk = [f"ssall{i}" for i in range(NOT_)]
        P.op('act', lambda e: e.activation(out=ssall[:, :], in_=ssall[:, :], func=AF.Sqrt, scale=1.0 / 128, bias=eps_t[:, 0:1]),
             reads=ssk + ["eps_t"], writes=ssk)
        P.op('dve', lambda e: e.reciprocal(out=ssall[:, :], in_=ssall[:, :]), reads=ssk, writes=ssk)
        for i in range(NOT_):
            u = i % 2
            P.op('dve', lambda e, i=i, u=u: e.scalar_tensor_tensor(out=a1[u][:, :], in0=a2all[:, i, :], scalar=ssall[:, i:i + 1], in1=gm_bc[:, :],
                                                                   op0=ALU.mult, op1=ALU.mult),
                 reads=[f"a2all{i}", f"ssall{i}", "gm_bc"], writes=[f"a1_{u}"])
            P.op('dve', lambda e, i=i, u=u: e.tensor_tensor(out=abf[u][:, :], in0=a1[u][:, :], in1=o_sig[:, i, :], op=ALU.mult),
                 reads=[f"a1_{u}", "o_sig"], writes=[f"abf{u}"])
            P.op('pe', lambda e, i=i, u=u: e.transpose(out=pbf[:, i * 128:(i + 1) * 128], in_=abf[u][:, :], identity=ident_bf[:, :]),
                 reads=[f"abf{u}", "ident_bf"], writes=["pbf"])
        P.op('dve', lambda e: e.tensor_copy(out=hmT_h[:, :], in_=pbf[:, :]), reads=["pbf"], writes=["aT_h"])
        P.op('sp', lambda e, h=h: e.dma_start(out=catT_d[8 + h, :, :], in_=hmT_h[:, :]), reads=["aT_h"], writes=["catT_d"], dma=True)
        next_kd[0] = load_w(1024 + (h + 1) * 128) if h + 1 < 8 else None
        P.flush()
    P.barrier()
    P.flush()
    if DEBUG:
        P.op('pool', lambda e: e.dma_start(out=dbg["cat"][:, :, :], in_=catT_d[:, :, :]), reads=["catT_d"], dma=True)
        P.op('pool', lambda e: e.dma_start(out=dbg["bias"][:, :], in_=biasd[:, :]), reads=["biasd"], dma=True)
        P.barrier()
        P.flush()


def head_norm_T(P, eps_t, src, srck, sm, smk, junk, g_bc, gk, h, mult, o_sig, oi, tmp, tmpk, obf, obfk, pbf, ident_bf, dstT, dstk, i):
    P.op('dve', lambda e: e.scalar_tensor_tensor(out=junk[:, :], in0=src[:, :], scalar=1.0, in1=src[:, :], op0=ALU.mult, op1=ALU.mult,
                                                 accum_out=sm[:, 4:5]),
         reads=[srck, smk], writes=["junk3", smk])
    P.op('act', lambda e: e.activation(out=sm[:, 4:5], in_=sm[:, 4:5], func=AF.Sqrt, scale=1.0 / 128, bias=eps_t[:, 0:1]),
         reads=[smk, "eps_t"], writes=[smk])
    P.op('dve', lambda e: e.reciprocal(out=sm[:, 4:5], in_=sm[:, 4:5]), reads=[smk], writes=[smk])
    if mult != 1.0:
        P.op('dve', lambda e: e.tensor_scalar(out=sm[:, 4:5], in0=sm[:, 4:5], scalar1=mult, scalar2=None, op0=ALU.mult),
             reads=[smk], writes=[smk])
    if o_sig is None:
        P.op('dve', lambda e: e.scalar_tensor_tensor(out=obf[:, :], in0=src[:, :], scalar=sm[:, 4:5], in1=g_bc[:, :],
                                                     op0=ALU.mult, op1=ALU.mult),
             reads=[srck, smk, gk], writes=[obfk])
    else:
        P.op('dve', lambda e: e.scalar_tensor_tensor(out=tmp[:, :], in0=src[:, :], scalar=sm[:, 4:5], in1=g_bc[:, :],
                                                     op0=ALU.mult, op1=ALU.mult),
             reads=[srck, smk, gk], writes=[tmpk])
        P.op('dve', lambda e: e.tensor_tensor(out=obf[:, :], in0=tmp[:, :], in1=o_sig[:, oi, :], op=ALU.mult),
             reads=[tmpk, "o_sig"], writes=[obfk])
    P.op('pe', lambda e: e.transpose(out=pbf[:, 0:128], in_=obf[:, :], identity=ident_bf[:, :]), reads=[obfk, "ident_bf"], writes=["pbf"])
    P.op('act', lambda e: e.copy(out=dstT[:, i * 128:(i + 1) * 128], in_=pbf[:, 0:128]), reads=["pbf"], writes=[dstk])


def conv_silu(P, upre, uacc, cw, cb, g, n, dst, dstk):
    P.op('dve', lambda e: e.tensor_scalar(out=uacc[:, 0:n], in0=upre[:, 1:1 + n], scalar1=cw[:, g, 0:1], scalar2=cb[:, g:g + 1],
                                          op0=ALU.mult, op1=ALU.add),
         reads=["upre", "cw", "cb"], writes=["uacc"])
    for j in range(1, 4):
        P.op('dve', lambda e, j=j: e.scalar_tensor_tensor(out=uacc[:, 0:n], in0=upre[:, 1 + j:1 + j + n], scalar=cw[:, g, j:j + 1],
                                                          in1=uacc[:, 0:n], op0=ALU.mult, op1=ALU.add),
             reads=["upre", "cw", "uacc"], writes=["uacc"])
    P.op('act', lambda e: e.activation(out=dst[:, 0:n], in_=uacc[:, 0:n], func=AF.Silu), reads=["uacc"], writes=[dstk])


def ffn_phase(nc, P, top, L):
    eps_t = L["eps_t"]
    banks = L["banks"]; pbf = L["pbf"]; ident_f = L["ident_f"]; sb = L["sb"]; dbg = L["dbg"]
    xs = L["xs"]; w_out = L["w_out"]; catT_d = L["catT_d"]; ln_ffn_g = L["ln_ffn_g"]; ln_f_g = L["ln_f_g"]
    w_rt = L["w_rt"]; b_rt = L["b_rt"]; w_gate = L["w_gate"]; w_up = L["w_up"]; w_down = L["w_down"]; y = L["y"]
    brr = [0]

    def nb():
        b = brr[0] % 7
        brr[0] += 1
        return b
    with ExitStack() as es:
        acc = sb(es, "acc", [128, NOT_, D], F32)
        x2T = sb(es, "x2T", [128, 16, OWN], BF16)
        comb = sb(es, "comb", [128, NOT_, 32], F32)
        for i in range(NOT_):
            P.op('sp', lambda e, i=i: e.dma_start(out=acc[:, i, :], in_=xs[3072 + i * 128:3072 + (i + 1) * 128, :]), writes=[f"acc{i}"], dma=True)
        with ExitStack() as s4:
            catT = sb(s4, "catT", [128, 16, OWN], BF16)
            P.op('sp', lambda e: e.dma_start(out=catT[:, :, :], in_=catT_d[:, :, :].rearrange("k p t -> p k t")),
                 reads=["catT_d"], writes=["catT"], dma=True)
            wo = [sb(s4, f"wo{i}", [128, 16, 512], BF16) for i in range(2)]
            for n in range(4):
                wi = n % 2
                for q4 in range(4):
                    P.op('pool', lambda e, n=n, wi=wi, q4=q4: e.dma_start(
                        out=wo[wi][:, q4 * 4:(q4 + 1) * 4, :],
                        in_=w_out[q4 * 512:(q4 + 1) * 512, n * 512:(n + 1) * 512].rearrange("(k p) c -> p k c", p=128)),
                        writes=[f"wo{wi}"], dma=True)
                for i in range(NOT_):
                    bk = nb()
                    for kc in range(16):
                        P.op('pe', lambda e, kc=kc, bk=bk, i=i, wi=wi: e.matmul(banks[bk][:, :], lhsT=catT[:, kc, i * 128:(i + 1) * 128],
                                                                               rhs=wo[wi][:, kc, :], start=(kc == 0), stop=(kc == 15)),
                             reads=["catT", f"wo{wi}"], writes=[f"bank{bk}"])
                    P.op('dve', lambda e, bk=bk, i=i, n=n: e.tensor_tensor(out=acc[:, i, n * 512:(n + 1) * 512], in0=banks[bk][:, :],
                                                                           in1=acc[:, i, n * 512:(n + 1) * 512], op=ALU.add),
                         reads=[f"bank{bk}", f"acc{i}"], writes=[f"acc{i}"])
            if DEBUG:
                for i in range(NOT_):
                    P.op('sp', lambda e, i=i: e.dma_start(out=dbg["x1"][i * 128:(i + 1) * 128, :], in_=acc[:, i, :]), reads=[f"acc{i}"], dma=True)
            P.barrier()
            P.flush()
        if STAGES < 5:
            return
        with ExitStack() as s5:
            g2 = sb(s5, "g2_bc", [128, D], F32)
            P.op('sp', lambda e: e.dma_start(out=g2[:, :], in_=bc_ap(ln_ffn_g, D)), writes=["g2"], dma=True)
            wr = sb(s5, "wr", [128, 16, 36], F32)
            for q4 in range(4):
                P.op('sp', lambda e, q4=q4: e.dma_start(out=wr[:, q4 * 4:(q4 + 1) * 4, :],
                                                        in_=w_rt[q4 * 512:(q4 + 1) * 512, :].rearrange("(k p) c -> p k c", p=128)),
                     writes=["wr"], dma=True)
            rb = sb(s5, "rb", [128, 36], F32)
            P.op('sp', lambda e: e.dma_start(out=rb[:, :], in_=bc_ap(b_rt, 36)), writes=["rb"], dma=True)
            x2f = sb(s5, "x2f", [128, D], F32)
            x2T32 = sb(s5, "x2T32", [128, 16, 128], F32)
            junk = sb(s5, "junk5", [128, D], BF16)
            sm = sb(s5, "sm5", [128, 16], F32)
            lg = sb(s5, "lg", [128, 36], F32)
            gmask = sb(s5, "gmask", [128, 4], F32)
            ge = sb(s5, "ge", [128, 4], F32)
            els = sb(s5, "els", [128, 8], F32)
            el2 = sb(s5, "el2", [128, 8], F32)
            mk1 = sb(s5, "mk1", [128, 8], F32)
            mk2 = sb(s5, "mk2", [128, 8], F32)
            ew = sb(s5, "ew", [128, 8], F32)
            smn = sb(s5, "smn", [128, NOT_], F32)
            lg_all = sb(s5, "lg_all", [128, NOT_, 36], F32)
            x2f2 = [x2f, sb(s5, "x2f_b", [128, D], F32)]
            x2T32_2 = [x2T32, sb(s5, "x2T32_b", [128, 16, 128], F32)]
            for i in range(NOT_):
                P.op('act', lambda e, i=i: e.activation(out=junk[:, :], in_=acc[:, i, :], func=AF.Square, accum_out=smn[:, i:i + 1]),
                     reads=[f"acc{i}"], writes=["junk5", f"smn{i}"])
            smk = [f"smn{i}" for i in range(NOT_)]
            P.op('act', lambda e: e.activation(out=smn[:, :], in_=smn[:, :], func=AF.Sqrt, scale=1.0 / D, bias=eps_t[:, 0:1]),
                 reads=smk + ["eps_t"], writes=smk)
            P.op('dve', lambda e: e.reciprocal(out=smn[:, :], in_=smn[:, :]), reads=smk, writes=smk)
            for i in range(NOT_):
                ub_ = i % 2
                xf = x2f2[ub_]
                xt32 = x2T32_2[ub_]
                P.op('dve', lambda e, i=i, xf=xf: e.scalar_tensor_tensor(out=xf[:, :], in0=acc[:, i, :], scalar=smn[:, i:i + 1], in1=g2[:, :],
                                                                         op0=ALU.mult, op1=ALU.mult),
                     reads=[f"acc{i}", f"smn{i}", "g2"], writes=[f"x2f{ub_}"])
                for q4 in range(4):
                    bk = nb()
                    for j in range(4):
                        kc = q4 * 4 + j
                        P.op('pe', lambda e, bk=bk, j=j, kc=kc, xf=xf: e.transpose(out=banks[bk][:, j * 128:(j + 1) * 128], in_=xf[:, kc * 128:(kc + 1) * 128],
                                                                                   identity=ident_f[:, :]),
                             reads=[f"x2f{ub_}", "ident_f"], writes=[f"bank{bk}"])
                    P.op('act', lambda e, bk=bk, q4=q4, xt32=xt32: e.copy(out=xt32[:, q4 * 4:(q4 + 1) * 4, :], in_=banks[bk][:, :].rearrange("p (j c) -> p j c", j=4)),
                         reads=[f"bank{bk}"], writes=[f"x2T32{ub_}"])
                    P.op('dve', lambda e, q4=q4, i=i, xt32=xt32: e.tensor_copy(out=x2T[:, q4 * 4:(q4 + 1) * 4, i * 128:(i + 1) * 128],
                                                                               in_=xt32[:, q4 * 4:(q4 + 1) * 4, :]),
                         reads=[f"x2T32{ub_}"], writes=["x2T"])
                bk = nb()
                for kc in range(16):
                    P.op('pe', lambda e, bk=bk, kc=kc, xt32=xt32: e.matmul(banks[bk][:, 0:36], lhsT=xt32[:, kc, :], rhs=wr[:, kc, :], start=(kc == 0), stop=(kc == 15)),
                         reads=[f"x2T32{ub_}", "wr"], writes=[f"bank{bk}"])
                P.op('dve', lambda e, bk=bk, i=i: e.tensor_tensor(out=lg_all[:, i, :], in0=banks[bk][:, 0:36], in1=rb[:, :], op=ALU.add),
                     reads=[f"bank{bk}", "rb"], writes=[f"lg{i}"])
            for i in range(NOT_):
                R = ["gmask", "ge", "els", "el2", "mk1", "mk2", "ew", "sm5", f"lg{i}"]
                lg = lg_all[:, i, :]
                if STAGES < 5.3:
                    continue
                P.op('dve', lambda e, lg=lg: e.tensor_reduce(out=sm[:, 1:2], in_=lg[:, 0:4], axis=AX.X, op=ALU.max), reads=R, writes=["sm5"])
                P.op('dve', lambda e, lg=lg: e.tensor_scalar(out=gmask[:, :], in0=lg[:, 0:4], scalar1=sm[:, 1:2], scalar2=None, op0=ALU.is_equal),
                     reads=R, writes=["gmask"])
                P.op('dve', lambda e: e.tensor_scalar(out=sm[:, 2:3], in0=sm[:, 1:2], scalar1=-1.0, scalar2=None, op0=ALU.mult), reads=R, writes=["sm5"])
                P.op('act', lambda e, lg=lg: e.activation(out=ge[:, :], in_=lg[:, 0:4], func=AF.Exp, bias=sm[:, 2:3], scale=1.0, accum_out=sm[:, 3:4]),
                     reads=R, writes=["ge", "sm5"])
                P.op('dve', lambda e: e.reciprocal(out=sm[:, 3:4], in_=sm[:, 3:4]), reads=R, writes=["sm5"])
                P.op('dve', lambda e, lg=lg: e.tensor_scalar(out=els[:, :], in0=lg[:, 4:12], scalar1=gmask[:, 0:1], scalar2=None, op0=ALU.mult), reads=R, writes=["els"])
                for g in range(1, 4):
                    P.op('dve', lambda e, g=g, lg=lg: e.scalar_tensor_tensor(out=els[:, :], in0=lg[:, 4 + 8 * g:12 + 8 * g], scalar=gmask[:, g:g + 1],
                                                                      in1=els[:, :], op0=ALU.mult, op1=ALU.add), reads=R, writes=["els"])
                P.op('dve', lambda e: e.tensor_reduce(out=sm[:, 4:5], in_=els[:, :], axis=AX.X, op=ALU.max), reads=R, writes=["sm5"])
                P.op('dve', lambda e: e.tensor_scalar(out=mk1[:, :], in0=els[:, :], scalar1=sm[:, 4:5], scalar2=None, op0=ALU.is_equal), reads=R, writes=["mk1"])
                P.op('dve', lambda e: e.scalar_tensor_tensor(out=el2[:, :], in0=mk1[:, :], scalar=-1e30, in1=els[:, :], op0=ALU.mult, op1=ALU.add),
                     reads=R, writes=["el2"])
                P.op('dve', lambda e: e.tensor_reduce(out=sm[:, 5:6], in_=el2[:, :], axis=AX.X, op=ALU.max), reads=R, writes=["sm5"])
                P.op('dve', lambda e: e.tensor_scalar(out=mk2[:, :], in0=el2[:, :], scalar1=sm[:, 5:6], scalar2=None, op0=ALU.is_equal), reads=R, writes=["mk2"])
                P.op('dve', lambda e: e.tensor_tensor(out=sm[:, 6:7], in0=sm[:, 5:6], in1=sm[:, 4:5], op=ALU.subtract), reads=R, writes=["sm5"])
                P.op('act', lambda e: e.activation(out=sm[:, 6:7], in_=sm[:, 6:7], func=AF.Exp), reads=R, writes=["sm5"])
                P.op('dve', lambda e: e.tensor_scalar(out=sm[:, 7:8], in0=sm[:, 6:7], scalar1=1.0, scalar2=None, op0=ALU.add), reads=R, writes=["sm5"])
                P.op('dve', lambda e: e.reciprocal(out=sm[:, 7:8], in_=sm[:, 7:8]), reads=R, writes=["sm5"])
                P.op('dve', lambda e: e.tensor_tensor(out=sm[:, 8:9], in0=sm[:, 6:7], in1=sm[:, 7:8], op=ALU.mult), reads=R, writes=["sm5"])
                P.op('dve', lambda e: e.tensor_tensor(out=sm[:, 7:8], in0=sm[:, 7:8], in1=sm[:, 3:4], op=ALU.mult), reads=R, writes=["sm5"])
                P.op('dve', lambda e: e.tensor_tensor(out=sm[:, 8:9], in0=sm[:, 8:9], in1=sm[:, 3:4], op=ALU.mult), reads=R, writes=["sm5"])
                P.op('dve', lambda e: e.tensor_scalar(out=ew[:, :], in0=mk1[:, :], scalar1=sm[:, 7:8], scalar2=None, op0=ALU.mult), reads=R, writes=["ew"])
                P.op('dve', lambda e: e.scalar_tensor_tensor(out=ew[:, :], in0=mk2[:, :], scalar=sm[:, 8:9], in1=ew[:, :], op0=ALU.mult, op1=ALU.add),
                     reads=R, writes=["ew"])
                for g in range(4):
                    P.op('dve', lambda e, g=g, i=i: e.tensor_scalar(out=comb[:, i, g * 8:(g + 1) * 8], in0=ew[:, :], scalar1=gmask[:, g:g + 1], scalar2=None,
                                                                    op0=ALU.mult), reads=R, writes=["comb"])
            P.barrier()
            P.flush()
        if STAGES < 6:
            return
        with ExitStack() as s6:
            NSLOT = 5
            slots = [sb(s6, f"wsl{i}", [128, 8192], BF16) for i in range(NSLOT)]
            hid = [sb(s6, f"hid{i}", [128, 4, OWN], BF16) for i in range(1)]
            sg = [sb(s6, f"sg{i}", [128, 512], BF16) for i in range(2)]
            print("SBUF remaining in experts:", nc.sbuf_bytes_remaining)
            def wviews(ex):
                sl = [(3 * ex + j) % NSLOT for j in range(3)]
                wgv = slots[sl[0]][:, :].rearrange("p (k f) -> p k f", k=16)
                wuv = slots[sl[1]][:, :].rearrange("p (k f) -> p k f", k=16)
                wdv = slots[sl[2]][:, :].rearrange("p (k f) -> p k f", k=4)
                return wgv, wuv, wdv, f"wsl{sl[0]}", f"wsl{sl[1]}", f"wsl{sl[2]}"

            def load_expert(ex):
                wgv, wuv, wdv, wgk, wuk, wdk = wviews(ex)
                for q4 in range(4):
                    P.op('pool', lambda e, ex=ex, q4=q4, wgv=wgv: e.dma_start(
                        out=wgv[:, q4 * 4:(q4 + 1) * 4, :], in_=w_gate[ex, q4 * 512:(q4 + 1) * 512, :].rearrange("(k p) f -> p k f", p=128)),
                        writes=[wgk], dma=True)
                for q4 in range(4):
                    P.op('pool', lambda e, ex=ex, q4=q4, wuv=wuv: e.dma_start(
                        out=wuv[:, q4 * 4:(q4 + 1) * 4, :], in_=w_up[ex, q4 * 512:(q4 + 1) * 512, :].rearrange("(k p) f -> p k f", p=128)),
                        writes=[wuk], dma=True)
                for q4 in range(4):
                    P.op('pool', lambda e, ex=ex, q4=q4, wdv=wdv: e.dma_start(
                        out=wdv[:, q4:q4 + 1, :], in_=w_down[ex, q4 * 128:(q4 + 1) * 128, :].rearrange("(k p) f -> p k f", p=128)),
                        writes=[wdk], dma=True)

            load_expert(0)
            for ex in range(32):
                wgv, wuv, wdv, wgk, wuk, wdk = wviews(ex)
                for fc in range(4):
                    for th in range(2):
                        gb_ = nb()
                        for kc in range(16):
                            P.op('pe', lambda e, gb_=gb_, kc=kc, fc=fc, th=th, wgv=wgv: e.matmul(banks[gb_][:, :], lhsT=wgv[:, kc, fc * 128:(fc + 1) * 128],
                                                                                            rhs=x2T[:, kc, th * 512:(th + 1) * 512], start=(kc == 0), stop=(kc == 15)),
                                 reads=[wgk, "x2T"], writes=[f"bank{gb_}"])
                        ub_ = nb()
                        for kc in range(16):
                            P.op('pe', lambda e, ub_=ub_, kc=kc, fc=fc, th=th, wuv=wuv: e.matmul(banks[ub_][:, :], lhsT=wuv[:, kc, fc * 128:(fc + 1) * 128],
                                                                                            rhs=x2T[:, kc, th * 512:(th + 1) * 512], start=(kc == 0), stop=(kc == 15)),
                                 reads=[wuk, "x2T"], writes=[f"bank{ub_}"])
                        si = (fc * 2 + th) % 2
                        P.op('act', lambda e, gb_=gb_, si=si: e.activation(out=sg[si][:, :], in_=banks[gb_][:, :], func=AF.Silu),
                             reads=[f"bank{gb_}"], writes=[f"sg{si}"])
                        P.op('dve', lambda e, ub_=ub_, si=si, fc=fc, th=th: e.tensor_tensor(out=hid[0][:, fc, th * 512:(th + 1) * 512], in0=banks[ub_][:, :],
                                                                                           in1=sg[si][:, :], op=ALU.mult),
                             reads=[f"bank{ub_}", f"sg{si}"], writes=["hid"])
                if ex + 1 < 32:
                    load_expert(ex + 1)
                for i in range(NOT_):
                    for n in range(4):
                        bk = nb()
                        for fc in range(4):
                            P.op('pe', lambda e, bk=bk, fc=fc, i=i, n=n, wdv=wdv: e.matmul(banks[bk][:, :], lhsT=hid[0][:, fc, i * 128:(i + 1) * 128],
                                                                                      rhs=wdv[:, fc, n * 512:(n + 1) * 512], start=(fc == 0), stop=(fc == 3)),
                                 reads=["hid", wdk], writes=[f"bank{bk}"])
                        P.op('dve', lambda e, bk=bk, i=i, n=n, ex=ex: e.scalar_tensor_tensor(out=acc[:, i, n * 512:(n + 1) * 512], in0=banks[bk][:, :],
                                                                                            scalar=comb[:, i, ex:ex + 1], in1=acc[:, i, n * 512:(n + 1) * 512],
                                                                                            op0=ALU.mult, op1=ALU.add),
                             reads=[f"bank{bk}", "comb", f"acc{i}"], writes=[f"acc{i}"])
                if ex % 8 == 7:
                    P.flush()
            P.barrier()
            P.flush()
        if STAGES < 7:
            return
        with ExitStack() as s7:
            g3 = sb(s7, "g3_bc", [128, D], F32)
            P.op('sp', lambda e: e.dma_start(out=g3[:, :], in_=bc_ap(ln_f_g, D)), writes=["g3"], dma=True)
            junk = sb(s7, "junk7", [128, D], BF16)
            sm = sb(s7, "sm7", [128, NOT_], F32)
            ob = [sb(s7, f"ob{i}", [128, D], F32) for i in range(2)]
            for i in range(NOT_):
                u = i % 2
                P.op('act', lambda e, i=i: e.activation(out=junk[:, :], in_=acc[:, i, :], func=AF.Square, accum_out=sm[:, i:i + 1]),
                     reads=[f"acc{i}"], writes=["junk7", f"sm7_{i}"])
                P.op('act', lambda e, i=i: e.activation(out=sm[:, i:i + 1], in_=sm[:, i:i + 1], func=AF.Sqrt, scale=1.0 / D, bias=eps_t[:, 0:1]),
                     reads=[f"sm7_{i}", "eps_t"], writes=[f"sm7_{i}"])
                P.op('dve', lambda e, i=i: e.reciprocal(out=sm[:, i:i + 1], in_=sm[:, i:i + 1]), reads=[f"sm7_{i}"], writes=[f"sm7_{i}"])
                P.op('dve', lambda e, i=i, u=u: e.scalar_tensor_tensor(out=ob[u][:, :], in0=acc[:, i, :], scalar=sm[:, i:i + 1], in1=g3[:, :],
                                                                       op0=ALU.mult, op1=ALU.mult),
                     reads=[f"acc{i}", f"sm7_{i}", "g3"], writes=[f"ob{u}"])
                P.op('sp', lambda e, i=i, u=u: e.dma_start(out=y[i * 128:(i + 1) * 128, :], in_=ob[u][:, :]), reads=[f"ob{u}"], writes=["y"], dma=True)
            P.final_wait('sp')
            P.flush()


_CACHE = {}


def _consts():
    if "c" in _CACHE:
        return _CACHE["c"]
    bt = _bucket_table()
    E = np.zeros((33, 2, 128, 128), np.float32)
    k = np.arange(128)[:, None]
    q = np.arange(128)[None, :]
    for di, d in enumerate((-1, 0)):
        rel = 128 * d + k - q
        bk = bt[rel + 255]
        for b in range(32):
            E[b, di] = (bk == b)
        if d == 0:
            E[32, di] = ((k // 64) > (q // 64))
    tri = np.zeros((128, 3, 128), np.float32)
    lp = np.arange(128)[:, None]
    l = np.arange(128)[None, :]
    tri[:, 0, :] = ((lp // 64) == (l // 64)) & (lp <= l)
    tri[:, 1, :] = (lp < 64) * np.ones((1, 128))
    tri[:, 2, :] = (lp >= 64) * np.ones((1, 128))
    cmask = (((lp // 64) == (l // 64)) & (lp <= l)).astype(np.float32)
    c = dict(Econst=E.reshape(33, -1), ident_bf=np.eye(128, dtype=np.float32).astype(ml_dtypes.bfloat16),
             ident_f=np.eye(128, dtype=np.float32), tri=tri.reshape(128, 384), cmask=cmask)
    _CACHE["c"] = c
    return c


def make_in_maps(x, rel_bias, ln_mix_g, w_in, conv_w, conv_b, b_i, b_f, lam_q1, lam_k1, lam_q2, lam_k2, diff_norm_g,
                 mlstm_norm_g, w_out, ln_ffn_g, w_group, b_group, w_router, b_router, w_gate, w_up, w_down, ln_f_g):
    f = lambda a: np.ascontiguousarray(np.asarray(a, dtype=np.float32))
    x = f(x)
    c = _consts()
    w_rt = np.concatenate([f(w_group)[0], f(w_router)[0].transpose(1, 0, 2).reshape(D, 32)], axis=1)
    b_rt = np.concatenate([f(b_group)[0], f(b_router)[0].reshape(32)])
    relb = np.concatenate([f(rel_bias), np.full((1, 8), -30000.0, np.float32)], axis=0)
    shared = dict(
        w_in=f(w_in)[0], w_out=f(w_out)[0], w_gate=f(w_gate)[0].reshape(32, D, 512), w_up=f(w_up)[0].reshape(32, D, 512),
        w_down=f(w_down)[0].reshape(32, 512, D), w_rt=np.ascontiguousarray(w_rt), b_rt=b_rt,
        ln_mix_g=f(ln_mix_g)[0], ln_ffn_g=f(ln_ffn_g)[0], ln_f_g=f(ln_f_g),
        conv_wt=np.ascontiguousarray(f(conv_w)[0].T.reshape(16, 128, 4).transpose(1, 0, 2).reshape(128, 64)),
        conv_b=np.ascontiguousarray(f(conv_b)[0].reshape(16, 128).T), b_if=np.concatenate([f(b_i)[0], f(b_f)[0]]),
        lam_v=np.stack([f(lam_q1)[0], f(lam_k1)[0], f(lam_q2)[0], f(lam_k2)[0]]),
        diff_norm_g=f(diff_norm_g)[0], mlstm_norm_g=f(mlstm_norm_g)[0], relb=relb, **c)
    maps = []
    for core in range(8):
        b, j = core // 4, core % 4
        pad = 3072 - 1024 * j
        xsl = np.zeros((S, D), np.float32)
        xsl[pad:] = x[b, :1024 * (j + 1)]
        v = np.zeros(S, np.float32)
        v[pad:] = 1.0
        m = dict(shared)
        m["xs"] = xsl
        m["valid"] = np.ascontiguousarray(v.reshape(NT, 128).T)
        maps.append(m)
    return maps


def kernel(**inputs):
    if "nc" not in _CACHE:
        _CACHE["nc"] = build_program()
    nc = _CACHE["nc"]
    maps = make_in_maps(**inputs)
    res = run_bass_kernel_spmd(nc, maps, core_ids=list(range(8)))
    _CACHE["res"] = res
    out = np.zeros((2, S, D), np.float32)
    for core in range(8):
        b, j = core // 4, core % 4
        out[b, j * 1024:(j + 1) * 1024] = np.asarray(res.results[core]["y"])
    return out
```

```python
import math
from contextlib import ExitStack
import numpy as np
import ml_dtypes
import concourse.bass as bass
import concourse.mybir as mybir
from concourse.bass_utils import run_bass_kernel_spmd

F32 = mybir.dt.float32
BF16 = mybir.dt.bfloat16
ALU = mybir.AluOpType
AF = mybir.ActivationFunctionType
AX = mybir.AxisListType

D = 2048
S = 4096
OWN = 1024
NT = 32
NOT_ = 8
T0 = 24
EPS = 1e-6
LAMBDA_INIT = 0.8 - 0.6 * math.exp(0.0)
DEBUG = False
STAGES = 99

def _bucket_table():
    half, max_exact = 16, 8
    rel = np.arange(-255, 256, dtype=np.int32)
    ret = np.where(rel > 0, half, 0)
    n = np.abs(rel)
    nf = np.maximum(n, 1).astype(np.float32)
    large = max_exact + (np.log(nf / np.float32(max_exact)) / np.float32(math.log(128 / max_exact))
                         * np.float32(half - max_exact)).astype(np.int32)
    large = np.minimum(large, half - 1)
    return ret + np.where(n < max_exact, n, large)


class Prog:
    LIM_C = 30000
    LIM_D = 30000

    def __init__(self, nc):
        self.nc = nc
        self.eng = dict(pe=nc.tensor, dve=nc.vector, act=nc.scalar, pool=nc.gpsimd, sp=nc.sync)
        self.q = {k: [] for k in self.eng}
        self.cur = {}
        self.lastw = {}
        self.readers = {}
        self.waited = {}
        self.pending_barrier = {k: [] for k in self.eng}
        self.nsem = 0
        self.dcount = {}

    NDSLOT = 8

    def _tick(self, stream, inc):
        extra = None
        if stream[0] == 'd':
            n = self.dcount.get(stream, 0)
            self.dcount[stream] = n + 1
            skey = (stream[0], stream[1], n % self.NDSLOT)
            if skey not in self.cur or self.cur[skey][1] + inc > self.LIM_D:
                if skey in self.cur:
                    extra = (self.cur[skey][0], self.cur[skey][1])
                sem = self.nc.alloc_semaphore(f"s{self.nsem}_d_{stream[1]}")
                self.nsem += 1
                self.cur[skey] = [sem, 0]
            c = self.cur[skey]
            if c[1] > 0:
                extra = (c[0], c[1])
            c[1] += inc
            return (c[0], c[1]), extra, skey
        if stream not in self.cur or self.cur[stream][1] + inc > self.LIM_C:
            sem = self.nc.alloc_semaphore(f"s{self.nsem}_{stream[0]}_{stream[1]}")
            self.nsem += 1
            self.cur[stream] = [sem, 0]
        c = self.cur[stream]
        c[1] += inc
        return (c[0], c[1]), extra, stream

    def barrier(self):
        toks = [(c[0], c[1]) for c in self.cur.values() if c[1] > 0]
        for e in self.eng:
            self.pending_barrier[e] = list(toks)

    def op(self, eng, fn, reads=(), writes=(), dma=False):
        deps = {}

        def add(tok):
            sem, val, stream = tok
            if stream == ('c', 'pe') and eng == 'pe' and not dma:
                return
            k = id(sem)
            if k not in deps or deps[k][1] < val:
                deps[k] = (sem, val)
        for k in reads:
            if k in self.lastw:
                add(self.lastw[k])
        for k in writes:
            if k in self.lastw:
                add(self.lastw[k])
            for tok in self.readers.get(k, {}).values():
                add(tok)
        for (sem, val) in self.pending_barrier[eng]:
            k = id(sem)
            if k not in deps or deps[k][1] < val:
                deps[k] = (sem, val)
        self.pending_barrier[eng] = []
        stream = ('d' if dma else 'c', eng)
        (sem, val), extra, stream = self._tick(stream, 16 if dma else 1)
        if extra is not None:
            k = id(extra[0])
            if k not in deps or deps[k][1] < extra[1]:
                deps[k] = extra
        waits = []
        for k, (s, v) in deps.items():
            wk = (eng, k)
            if self.waited.get(wk, 0) >= v:
                continue
            self.waited[wk] = v
            waits.append((s, v))
        inc = 16 if dma else 1

        def emit(e, waits=waits, fn=fn, sem=sem, inc=inc):
            for (s, v) in waits:
                e.wait_ge(s, v)
            fn(e).then_inc(sem, inc)
        self.q[eng].append(emit)
        tok = (sem, val, stream)
        for k in reads:
            self.readers.setdefault(k, {})[stream] = tok
        for k in writes:
            self.lastw[k] = tok
            self.readers[k] = {}
        return tok

    def final_wait(self, eng):
        toks = [(c[0], c[1]) for c in self.cur.values() if c[1] > 0]

        def emit(e, toks=toks):
            for (s, v) in toks:
                e.wait_ge(s, v)
        self.q[eng].append(emit)

    def flush(self):
        nc = self.nc
        q = self.q
        self.q = {k: [] for k in self.eng}
        with nc.Block() as block:
            @block.tensor
            def _(e):
                for f in q['pe']:
                    f(e)

            @block.vector
            def _(e):
                for f in q['dve']:
                    f(e)

            @block.scalar
            def _(e):
                for f in q['act']:
                    f(e)

            @block.gpsimd
            def _(e):
                for f in q['pool']:
                    f(e)

            @block.sync
            def _(e):
                for f in q['sp']:
                    f(e)


def bc_ap(handle, n, offset=0, parts=128):
    return bass.AP(handle, offset, [[0, parts], [1, n]])


def build_program():
    nc = bass.Bass("TRN2", target_bir_lowering=False)
    P = Prog(nc)

    def din(name, shape, dt=F32):
        return nc.dram_tensor(name, list(shape), dt, kind="ExternalInput")

    xs = din("xs", [S, D])
    valid = din("valid", [128, NT])
    w_in = din("w_in", [D, 7184])
    w_out = din("w_out", [D, D])
    w_gate = din("w_gate", [32, D, 512])
    w_up = din("w_up", [32, D, 512])
    w_down = din("w_down", [32, 512, D])
    w_rt = din("w_rt", [D, 36])
    b_rt = din("b_rt", [36])
    ln_mix_g = din("ln_mix_g", [D])
    ln_ffn_g = din("ln_ffn_g", [D])
    ln_f_g = din("ln_f_g", [D])
    conv_wt = din("conv_wt", [128, 64])
    conv_b = din("conv_b", [128, 16])
    b_if = din("b_if", [16])
    lam_v = din("lam_v", [4, 64])
    dng = din("diff_norm_g", [1024])
    mng = din("mlstm_norm_g", [1024])
    relb = din("relb", [33, 8])
    Econst = din("Econst", [33, 2 * 128 * 128])
    ident_bf_d = din("ident_bf", [128, 128], BF16)
    ident_f_d = din("ident_f", [128, 128])
    tri_d = din("tri", [128, 3 * 128])
    cmask_d = din("cmask", [128, 128])
    y = nc.dram_tensor("y", [OWN, D], F32, kind="ExternalOutput")
    biasd = nc.dram_tensor("biasd", [8, 2 * 128 * 128], F32)
    catT_d = nc.dram_tensor("catT_d", [16, 128, OWN], BF16)
    dbg = {}
    if DEBUG:
        dbg["cat"] = nc.dram_tensor("dbg_cat", [16, 128, OWN], BF16, kind="ExternalOutput")
        dbg["x1"] = nc.dram_tensor("dbg_x1", [OWN, D], F32, kind="ExternalOutput")
        dbg["gates"] = nc.dram_tensor("dbg_gates", [128, 5 * 256], F32, kind="ExternalOutput")
        dbg["hT"] = nc.dram_tensor("dbg_hT", [128, 16 * 256], BF16, kind="ExternalOutput")
        dbg["bias"] = nc.dram_tensor("dbg_bias", [8, 2 * 128 * 128], F32, kind="ExternalOutput")
        dbg["o12"] = nc.dram_tensor("dbg_o12", [128, 2 * 129], F32, kind="ExternalOutput")
        dbg["pT"] = nc.dram_tensor("dbg_pT", [128, 512], BF16, kind="ExternalOutput")

    def sb(es, name, shape, dt):
        return es.enter_context(nc.sbuf_tensor("sb_" + name, list(shape), dt))

    def ps(es, name, shape, dt=F32):
        return es.enter_context(nc.psum_tensor(name, list(shape), dt))

    with ExitStack() as top:
        banks = [ps(top, f"bank{i}", [128, 512]) for i in range(7)]
        pbf = ps(top, "pbf", [128, 1024], BF16)
        ident_bf = sb(top, "ident_bf_s", [128, 128], BF16)
        ident_f = sb(top, "ident_f_s", [128, 128], F32)
        P.op('sp', lambda e: e.dma_start(out=ident_bf[:, :], in_=ident_bf_d[:, :]), writes=["ident_bf"], dma=True)
        P.op('sp', lambda e: e.dma_start(out=ident_f[:, :], in_=ident_f_d[:, :]), writes=["ident_f"], dma=True)

        eps_t = sb(top, "eps_t", [128, 1], F32)
        P.op('dve', lambda e: e.memset(eps_t[:, :], EPS), writes=["eps_t"])
        bank_rr = [0]

        def next_bank():
            b = bank_rr[0] % 7
            bank_rr[0] += 1
            return b

        with ExitStack() as mx:
            hT = sb(mx, "hT", [128, 16, S], BF16)
            alpha = sb(mx, "alpha", [128, 256], F32)
            alphac = sb(mx, "alphac", [128, 256], F32)
            beta = sb(mx, "beta", [128, 256], F32)
            egA = sb(mx, "egA", [128, 256], F32)
            egB = sb(mx, "egB", [128, 256], F32)
            valid_s = sb(mx, "valid_s", [128, NT], F32)
            P.op('sp', lambda e: e.dma_start(out=valid_s[:, :], in_=valid[:, :]), writes=["valid"], dma=True)

            with ExitStack() as s1:
                g_bc = sb(s1, "g_bc", [128, D], F32)
                P.op('sp', lambda e: e.dma_start(out=g_bc[:, :], in_=bc_ap(ln_mix_g, D)), writes=["g_bc"], dma=True)
                xb = [sb(s1, f"xb{i}", [128, D], F32) for i in range(2)]
                xn = [sb(s1, f"xn{i}", [128, D], BF16) for i in range(2)]
                junk = sb(s1, "junk1", [128, D], BF16)
                ss = sb(s1, "ss1", [128, NT], F32)
                rstd = sb(s1, "rstd1", [128, NT], F32)
                relb_s = sb(s1, "relb_s", [33, 8], F32)
                P.op('pool', lambda e: e.dma_start(out=relb_s[:, :], in_=relb[:, :]), writes=["relb_s"], dma=True)
                Es = [sb(s1, f"Es{i}", [33, 2048], F32) for i in range(2)]
                bstage = [sb(s1, f"bstage{i}", [8, 2048], F32) for i in range(2)]

                def bias_piece(pc):
                    u = pc % 2
                    P.op('pool', lambda e: e.dma_start(out=Es[u][:, :], in_=Econst[:, pc * 2048:(pc + 1) * 2048]),
                         writes=[f"Es{u}"], dma=True)
                    for c in range(4):
                        bk = c % 4
                        P.op('pe', lambda e, c=c, bk=bk: e.matmul(banks[bk][0:8, :], lhsT=relb_s[:, :], rhs=Es[u][:, c * 512:(c + 1) * 512],
                                                                  start=True, stop=True),
                             reads=["relb_s", f"Es{u}"], writes=[f"bank{bk}"])
                        P.op('dve', lambda e, c=c, bk=bk: e.tensor_copy(out=bstage[u][:, c * 512:(c + 1) * 512], in_=banks[bk][0:8, :]),
                             reads=[f"bank{bk}"], writes=[f"bstage{u}"])
                    P.op('pool', lambda e: e.dma_start(out=biasd[:, pc * 2048:(pc + 1) * 2048], in_=bstage[u][:, :]),
                         reads=[f"bstage{u}"], writes=["biasd"], dma=True)

                w_if = sb(s1, "w_if", [128, 16, 16], BF16)
                P.op('pool', lambda e: e.dma_start(out=w_if[:, :, :], in_=w_in[:, 7168:7184].rearrange("(k p) c -> p k c", p=128)),
                     writes=["w_if"], dma=True)
                gbk = banks[4]

                def gate_mm(t):
                    for kc in range(16):
                        P.op('pe', lambda e, t=t, kc=kc: e.matmul(gbk[:, t * 16:(t + 1) * 16], lhsT=hT[:, kc, t * 128:(t + 1) * 128],
                                                                  rhs=w_if[:, kc, :], start=(kc == 0), stop=(kc == 15)),
                             reads=[f"hT{t}", "w_if"], writes=["bank4"])

                for t in range(NT):
                    b = t % 2
                    if t % 2 == 1:
                        bias_piece(t // 2)
                    if t >= 1:
                        gate_mm(t - 1)
                    P.op('sp', lambda e, t=t, b=b: e.dma_start(out=xb[b][:, :], in_=xs[t * 128:(t + 1) * 128, :]),
                         writes=[f"xb{b}"], dma=True)
                    P.op('act', lambda e, t=t, b=b: e.activation(out=junk[:, :], in_=xb[b][:, :], func=AF.Square,
                                                                 accum_out=ss[:, t:t + 1]),
                         reads=[f"xb{b}"], writes=["junk1", f"ss{t}"])
                    P.op('act', lambda e, t=t: e.activation(out=rstd[:, t:t + 1], in_=ss[:, t:t + 1], func=AF.Sqrt, scale=1.0 / D, bias=eps_t[:, 0:1]),
                         reads=[f"ss{t}", "eps_t"], writes=[f"rstd{t}"])
                    P.op('dve', lambda e, t=t: e.reciprocal(out=rstd[:, t:t + 1], in_=rstd[:, t:t + 1]),
                         reads=[f"rstd{t}"], writes=[f"rstd{t}"])
                    P.op('dve', lambda e, t=t, b=b: e.scalar_tensor_tensor(out=xn[b][:, :], in0=xb[b][:, :],
                                                                           scalar=rstd[:, t:t + 1], in1=g_bc[:, :],
                                                                           op0=ALU.mult, op1=ALU.mult),
                         reads=[f"xb{b}", f"rstd{t}", "g_bc"], writes=[f"xn{b}"])
                    for half in range(2):
                        for kk in range(8):
                            kc = half * 8 + kk
                            P.op('pe', lambda e, kc=kc, kk=kk, b=b: e.transpose(out=pbf[:, kk * 128:(kk + 1) * 128],
                                                                                in_=xn[b][:, kc * 128:(kc + 1) * 128],
                                                                                identity=ident_bf[:, :]),
                                 reads=[f"xn{b}", "ident_bf"], writes=["pbf"])
                        eng = 'act' if half == 0 else 'dve'
                        if eng == 'act':
                            P.op('act', lambda e, t=t, half=half: e.copy(out=hT[:, half * 8:(half + 1) * 8, t * 128:(t + 1) * 128],
                                                                          in_=pbf[:, :].rearrange("p (k t) -> p k t", k=8)),
                                 reads=["pbf"], writes=[f"hT{t}"])
                        else:
                            P.op('dve', lambda e, t=t, half=half: e.tensor_copy(out=hT[:, half * 8:(half + 1) * 8, t * 128:(t + 1) * 128],
                                                                                 in_=pbf[:, :].rearrange("p (k t) -> p k t", k=8)),
                                 reads=["pbf"], writes=[f"hT{t}"])
                gate_mm(NT - 1)
                P.barrier()
                P.flush()
            if DEBUG:
                P.op('sp', lambda e: e.dma_start(out=dbg["hT"][:, :].rearrange("p (k t) -> p k t", k=16), in_=hT[:, :, 3072:3072 + 256]),
                     reads=[f"hT{t}" for t in range(NT)], dma=True)

            with ExitStack() as s2:
                gb = sb(s2, "gb", [128, NT, 16], F32)
                P.op('sp', lambda e: e.dma_start(out=gb[:, :, :], in_=bass.AP(b_if, 0, [[0, 128], [0, NT], [1, 16]])),
                     writes=["gb"], dma=True)
                tri = sb(s2, "tri", [128, 3, 128], F32)
                P.op('sp', lambda e: e.dma_start(out=tri[:, :, :], in_=tri_d[:, :].rearrange("p (a l) -> p a l", a=3)),
                     writes=["tri"], dma=True)
                gpre = sb(s2, "gpre", [128, NT, 16], F32)
                ipre = sb(s2, "ipre", [128, NT, 8], F32)
                spv = sb(s2, "spv", [128, NT, 8], F32)
                tmp = sb(s2, "tmpg", [128, 256], F32)
                gbk = banks[4]
                P.op('dve', lambda e: e.tensor_tensor(out=gpre[:, :, :], in0=gbk[:, :].rearrange("p (t c) -> p t c", c=16),
                                                      in1=gb[:, :, :], op=ALU.add),
                     reads=["bank4", "gb"], writes=["gpre"])
                P.op('dve', lambda e: e.tensor_copy(out=ipre[:, :, :], in_=gpre[:, :, 0:8]), reads=["gpre"], writes=["ipre"])
                P.op('act', lambda e: e.activation(out=spv[:, :, :], in_=gpre[:, :, 8:16], func=AF.Exp, scale=-1.0),
                     reads=["gpre"], writes=["spv"])
                P.op('act', lambda e: e.activation(out=spv[:, :, :], in_=spv[:, :, :], func=AF.Ln, bias=1.0, scale=1.0),
                     reads=["spv"], writes=["spv"])
                spf = spv[:, :, :].rearrange("p t c -> p (t c)")
                for a_i, bk in ((0, 1), (1, 2), (2, 3)):
                    P.op('pe', lambda e, a_i=a_i, bk=bk: e.matmul(banks[bk][:, 0:256], lhsT=tri[:, a_i, :], rhs=spf,
                                                                  start=True, stop=True),
                         reads=["tri", "spv"], writes=[f"bank{bk}"])
                P.op('act', lambda e: e.activation(out=alpha[:, :], in_=banks[1][:, 0:256], func=AF.Exp, scale=-1.0),
                     reads=["bank1"], writes=["alpha"])
                P.op('dve', lambda e: e.tensor_scalar(out=alphac[:, :], in0=alpha[:, :], scalar1=128.0 ** -0.5, scalar2=None,
                                                      op0=ALU.mult),
                     reads=["alpha"], writes=["alphac"])
                P.op('dve', lambda e: e.tensor_tensor(out=tmp[:, :], in0=banks[1][:, 0:256],
                                                      in1=ipre[:, :, :].rearrange("p t c -> p (t c)"), op=ALU.add),
                     reads=["bank1", "ipre", "alpha"], writes=["tmpg"])
                P.op('act', lambda e: e.activation(out=beta[:, :], in_=tmp[:, :], func=AF.Exp), reads=["tmpg"], writes=["beta"])
                P.op('act', lambda e: e.activation(out=egA[:, :], in_=banks[2][:, 0:256], func=AF.Exp, scale=-1.0),
                     reads=["bank2"], writes=["egA"])
                P.op('act', lambda e: e.activation(out=egB[:, :], in_=banks[3][:, 0:256], func=AF.Exp, scale=-1.0),
                     reads=["bank3"], writes=["egB"])
                if DEBUG:
                    for i_, tt in enumerate((alpha, beta, egA, egB, alphac)):
                        P.op('sp', lambda e, i_=i_, tt=tt: e.dma_start(out=dbg["gates"][:, i_ * 256:(i_ + 1) * 256], in_=tt[:, :]),
                             reads=["alpha", "beta", "egA", "egB", "alphac"], dma=True)
                P.barrier()
                P.flush()

            if STAGES >= 3:
                mixer_heads(nc, P, mx, locals())
        if STAGES >= 4:
            ffn_phase(nc, P, top, locals())
        P.final_wait('sp')
        P.final_wait('pool')
        P.flush()
    return nc


def mixer_heads(nc, P, mx, L):
    eps_t = L["eps_t"]
    hT = L["hT"]; banks = L["banks"]; pbf = L["pbf"]; ident_bf = L["ident_bf"]
    alpha = L["alpha"]; alphac = L["alphac"]; beta = L["beta"]; egA = L["egA"]; egB = L["egB"]
    valid_s = L["valid_s"]; w_in = L["w_in"]; sb = L["sb"]; dbg = L["dbg"]
    conv_wt = L["conv_wt"]; conv_b = L["conv_b"]; lam_v = L["lam_v"]; dng = L["dng"]; mng = L["mng"]
    relb = L["relb"]; Econst = L["Econst"]; biasd = L["biasd"]; catT_d = L["catT_d"]; cmask_d = L["cmask_d"]
    es = mx
    cw = sb(es, "cw", [128, 16, 4], F32)
    P.op('sp', lambda e: e.dma_start(out=cw[:, :, :], in_=conv_wt[:, :].rearrange("p (g j) -> p g j", j=4)), writes=["cw"], dma=True)
    cb = sb(es, "cb", [128, 16], F32)
    P.op('sp', lambda e: e.dma_start(out=cb[:, :], in_=conv_b[:, :]), writes=["cb"], dma=True)
    gd_bc = sb(es, "gd_bc", [128, 128], F32)
    gm_bc = sb(es, "gm_bc", [128, 128], F32)
    cmask = sb(es, "cmask", [128, 128], F32)
    P.op('sp', lambda e: e.dma_start(out=cmask[:, :], in_=cmask_d[:, :]), writes=["cmask"], dma=True)
    c15 = sb(es, "c15", [128, 8], F32)
    P.op('sp', lambda e: e.dma_start(out=c15[:, :], in_=bc_ap(relb, 8, offset=15 * 8)), writes=["c15"], dma=True)
    lamt = sb(es, "lamt", [128, 4, 64], F32)
    P.op('sp', lambda e: e.dma_start(out=lamt[:, :, :], in_=bass.AP(lam_v, 0, [[0, 128], [64, 4], [1, 64]])), writes=["lamt"], dma=True)
    lamp = sb(es, "lamp", [128, 2, 64], F32)
    lams = sb(es, "lams", [128, 2], F32)
    nlam = sb(es, "nlam", [128, 1], F32)
    P.op('dve', lambda e: e.tensor_tensor(out=lamp[:, 0, :], in0=lamt[:, 0, :], in1=lamt[:, 1, :], op=ALU.mult), reads=["lamt"], writes=["lamp"])
    P.op('dve', lambda e: e.tensor_tensor(out=lamp[:, 1, :], in0=lamt[:, 2, :], in1=lamt[:, 3, :], op=ALU.mult), reads=["lamt", "lamp"], writes=["lamp"])
    P.op('dve', lambda e: e.tensor_reduce(out=lams[:, :], in_=lamp[:, :, :], axis=AX.X, op=ALU.add), reads=["lamp"], writes=["lams"])
    P.op('act', lambda e: e.activation(out=lams[:, :], in_=lams[:, :], func=AF.Exp), reads=["lams"], writes=["lams"])
    P.op('dve', lambda e: e.tensor_tensor(out=nlam[:, :], in0=lams[:, 1:2], in1=lams[:, 0:1], op=ALU.subtract), reads=["lams"], writes=["nlam"])
    P.op('dve', lambda e: e.tensor_scalar(out=nlam[:, :], in0=nlam[:, :], scalar1=-LAMBDA_INIT, scalar2=None, op0=ALU.add), reads=["nlam"], writes=["nlam"])
    Bnear = sb(es, "Bnear", [128, 2, 128], F32)

    kT_d = sb(es, "kT_d", [128, S], BF16)
    qT_d = sb(es, "qT_d", [128, OWN], BF16)
    v_d = sb(es, "v_d", [128, NT, 129], BF16)
    kT_m = kT_d
    k_tok = sb(es, "k_tok", [128, NT, 128], BF16)
    Vp = v_d
    qT_m = qT_d
    qTA = sb(es, "qTA", [128, OWN], BF16)
    qTB = sb(es, "qTB", [128, OWN], BF16)
    o_sig = sb(es, "o_sig", [128, NOT_, 128], BF16)
    aT_h = sb(es, "aT_h", [128, OWN], BF16)
    hmT_h = aT_h
    ub = [sb(es, f"ub{i}", [128, 516], F32) for i in range(2)]
    uacc = [sb(es, f"uacc{i}", [128, 512], F32) for i in range(2)]
    wb = [sb(es, f"wb{i}", [128, 16, 128], BF16) for i in range(2)]
    pT = [sb(es, f"pT{i}", [128, 512], BF16) for i in range(4)]
    nrt = [sb(es, f"nrt{i}", [128, 256], F32) for i in range(2)]
    Z = sb(es, "Z", [128, 129], F32)
    CTb = [sb(es, f"CTb{i}", [128, 129], BF16) for i in range(16)]
    SM = [sb(es, f"SM{i}", [128, 128], BF16) for i in range(4)]
    smo = sb(es, "smo", [128, NOT_, 4], F32)
    sm1 = [sb(es, f"sm1_{i}", [128, 8], F32) for i in range(2)]
    a1 = [sb(es, f"a1_{i}", [128, 128], F32) for i in range(2)]
    a2 = [sb(es, f"a2_{i}", [128, 128], F32) for i in range(2)]
    abf = [sb(es, f"abf{i}", [128, 128], BF16) for i in range(2)]
    junk = sb(es, "junk3", [128, 128], BF16)
    a2all = sb(es, "a2all", [128, NOT_, 128], F32)
    ssall = sb(es, "ssall", [128, NOT_], F32)
    dbgt = None
    sb_small_qprev = sb(es, "qprev", [128, 4], F32)
    print("SBUF remaining after mixer alloc:", nc.sbuf_bytes_remaining)
    wrr = [0]
    brr = [0]
    srr = [0]

    def nb():
        b = brr[0] % 7
        brr[0] += 1
        return b

    def load_w(c0):
        i = wrr[0] % 2
        wrr[0] += 1
        for hf in range(2):
            P.op('pool', lambda e, i=i, c0=c0, hf=hf: e.dma_start(
                out=wb[i][:, hf * 8:(hf + 1) * 8, :],
                in_=w_in[hf * 1024:(hf + 1) * 1024, c0:c0 + 128].rearrange("(k p) c -> p k c", p=128)),
                writes=[f"wb{i}"], dma=True)
        return i

    def proj_fm(wi, tok0, n, evac):
        bk = nb()
        for kc in range(16):
            P.op('pe', lambda e, kc=kc, bk=bk: e.matmul(banks[bk][:, 0:n], lhsT=wb[wi][:, kc, :], rhs=hT[:, kc, tok0:tok0 + n],
                                                        start=(kc == 0), stop=(kc == 15)),
                 reads=[f"wb{wi}"] + [f"hT{t}" for t in range(tok0 // 128, (tok0 + n) // 128)], writes=[f"bank{bk}"])
        evac(bk)

    def proj_tm(wi, t0, nt, evac):
        bk = nb()
        for j in range(nt):
            t = t0 + j
            for kc in range(16):
                P.op('pe', lambda e, kc=kc, bk=bk, j=j, t=t: e.matmul(banks[bk][:, j * 128:(j + 1) * 128],
                                                                     lhsT=hT[:, kc, t * 128:(t + 1) * 128], rhs=wb[wi][:, kc, :],
                                                                     start=(kc == 0), stop=(kc == 15)),
                     reads=[f"wb{wi}", f"hT{t}"], writes=[f"bank{bk}"])
        evac(bk)

    next_kd = [None]
    for h in range(8):
        P.op('sp', lambda e, h=h: e.dma_start(out=gd_bc[:, :], in_=bc_ap(dng, 128, offset=h * 128)), writes=["gd_bc"], dma=True)
        P.op('sp', lambda e, h=h: e.dma_start(out=gm_bc[:, :], in_=bc_ap(mng, 128, offset=h * 128)), writes=["gm_bc"], dma=True)
        P.op('sp', lambda e, h=h: e.dma_start(out=Bnear[:, :, :], in_=biasd[h, :].rearrange("(d k q) -> k d q", d=2, k=128)),
             reads=["biasd"], writes=["Bnear"], dma=True)
        wi = next_kd[0] if next_kd[0] is not None else load_w(1024 + h * 128)
        for blk in range(8):
            proj_fm(wi, blk * 512, 512, lambda bk, blk=blk: P.op(
                'act', lambda e: e.copy(out=kT_d[:, blk * 512:(blk + 1) * 512], in_=banks[bk][:, :]),
                reads=[f"bank{bk}"], writes=["kT_d"]))
        wi = load_w(h * 128)
        for blk in range(2):
            proj_fm(wi, 3072 + blk * 512, 512, lambda bk, blk=blk: P.op(
                'dve', lambda e: e.tensor_copy(out=qT_d[:, blk * 512:(blk + 1) * 512], in_=banks[bk][:, :]),
                reads=[f"bank{bk}"], writes=["qT_d"]))
        P.op('dve', lambda e: e.tensor_copy(out=v_d[:, :, 128], in_=valid_s[:, :]), reads=["valid"], writes=["v_d"])
        wi = load_w(2048 + h * 128)
        for g4 in range(8):
            proj_tm(wi, g4 * 4, 4, lambda bk, g4=g4: P.op(
                'act', lambda e: e.copy(out=v_d[:, g4 * 4:(g4 + 1) * 4, 0:128], in_=banks[bk][:, :].rearrange("p (j c) -> p j c", j=4)),
                reads=[f"bank{bk}"], writes=["v_d"]))
        items = []
        for i in range(NOT_):
            qt = T0 + i
            far = list(range(0, qt - 1))
            groups = [far[j:j + 4] for j in range(0, len(far), 4)] + [[qt - 1, qt]]
            for gi, grp in enumerate(groups):
                items.append(dict(i=i, grp=grp, near=(gi == len(groups) - 1), first=(gi == 0)))

        def emit_qk(n):
            it = items[n]
            i = it["i"]
            for j, kt in enumerate(it["grp"]):
                for m in range(2):
                    sbk = 2 * (n % 2) + m
                    P.op('pe', lambda e, j=j, kt=kt, sbk=sbk, m=m, i=i: e.matmul(
                        banks[sbk][:, j * 128:(j + 1) * 128], lhsT=kT_d[m * 64:(m + 1) * 64, kt * 128:(kt + 1) * 128],
                        rhs=qT_d[m * 64:(m + 1) * 64, i * 128:(i + 1) * 128], start=True, stop=True),
                        reads=["kT_d", "qT_d"], writes=[f"bank{sbk}"])

        def emit_exp(n):
            it = items[n]
            nn = len(it["grp"]) * 128
            for m in range(2):
                sbk = 2 * (n % 2) + m
                pi = 2 * (n % 2) + m
                if not it["near"]:
                    P.op('act', lambda e, sbk=sbk, nn=nn, pi=pi, h=h: e.activation(out=pT[pi][:, 0:nn], in_=banks[sbk][:, 0:nn], func=AF.Exp,
                                                                                 scale=0.125, bias=c15[:, h:h + 1]),
                         reads=[f"bank{sbk}", "c15"], writes=[f"pT{pi}"])
                else:
                    P.op('dve', lambda e, sbk=sbk, h=h, m=m: e.scalar_tensor_tensor(
                        out=nrt[m][:, :], in0=banks[sbk][:, 0:256], scalar=0.125,
                        in1=Bnear[:, :, :].rearrange("k d q -> k (d q)"), op0=ALU.mult, op1=ALU.add),
                        reads=[f"bank{sbk}", "Bnear"], writes=[f"nrt{m}"])
                    P.op('act', lambda e, pi=pi, m=m: e.activation(out=pT[pi][:, 0:256], in_=nrt[m][:, :], func=AF.Exp),
                         reads=[f"nrt{m}"], writes=[f"pT{pi}"])

        def emit_pv(n):
            it = items[n]
            i = it["i"]
            for m in range(2):
                pi = 2 * (n % 2) + m
                obk = 4 + m
                for j, kt in enumerate(it["grp"]):
                    first = it["first"] and j == 0
                    last = it["near"] and (j == len(it["grp"]) - 1)
                    P.op('pe', lambda e, j=j, kt=kt, pi=pi, obk=obk, first=first, last=last: e.matmul(
                        banks[obk][:, 0:129], lhsT=pT[pi][:, j * 128:(j + 1) * 128], rhs=v_d[:, kt, :], start=first, stop=last),
                        reads=[f"pT{pi}", "v_d"], writes=[f"bank{obk}"])
            if it["near"]:
                combine(i)

        def combine(i):
            u = i % 2
            ob = [4, 5]
            o1, o2 = banks[ob[0]], banks[ob[1]]
            P.op('dve', lambda e, u=u, o1=o1: e.reciprocal(out=sm1[u][:, 0:1], in_=o1[:, 128:129]), reads=[f"bank{ob[0]}"], writes=[f"sm1_{u}"])
            P.op('dve', lambda e, u=u, o2=o2: e.reciprocal(out=sm1[u][:, 1:2], in_=o2[:, 128:129]), reads=[f"bank{ob[1]}", f"sm1_{u}"], writes=[f"sm1_{u}"])
            P.op('dve', lambda e, u=u: e.tensor_tensor(out=sm1[u][:, 1:2], in0=sm1[u][:, 1:2], in1=nlam[:, 0:1], op=ALU.mult),
                 reads=[f"sm1_{u}", "nlam"], writes=[f"sm1_{u}"])
            P.op('dve', lambda e, u=u, o1=o1: e.tensor_scalar(out=a1[u][:, :], in0=o1[:, 0:128], scalar1=sm1[u][:, 0:1], scalar2=None, op0=ALU.mult),
                 reads=[f"bank{ob[0]}", f"sm1_{u}"], writes=[f"a1_{u}"])
            P.op('dve', lambda e, u=u, o2=o2, i=i: e.scalar_tensor_tensor(out=a2all[:, i, :], in0=o2[:, 0:128], scalar=sm1[u][:, 1:2], in1=a1[u][:, :],
                                                                          op0=ALU.mult, op1=ALU.add),
                 reads=[f"bank{ob[1]}", f"sm1_{u}", f"a1_{u}"], writes=[f"a2all{i}"])
            P.op('dve', lambda e, i=i: e.scalar_tensor_tensor(out=junk[:, :], in0=a2all[:, i, :], scalar=1.0, in1=a2all[:, i, :],
                                                              op0=ALU.mult, op1=ALU.mult, accum_out=ssall[:, i:i + 1]),
                 reads=[f"a2all{i}", f"ssall{i}"], writes=["junk3", f"ssall{i}"])

        def finish_attn():
            ssk = [f"ssall{i}" for i in range(NOT_)]
            P.op('act', lambda e: e.activation(out=ssall[:, :], in_=ssall[:, :], func=AF.Sqrt, scale=1.0 / 128, bias=eps_t[:, 0:1]),
                 reads=ssk + ["eps_t"], writes=ssk)
            P.op('dve', lambda e: e.reciprocal(out=ssall[:, :], in_=ssall[:, :]), reads=ssk, writes=ssk)
            P.op('dve', lambda e: e.tensor_scalar(out=ssall[:, :], in0=ssall[:, :], scalar1=(1.0 - LAMBDA_INIT), scalar2=None, op0=ALU.mult),
                 reads=ssk, writes=ssk)

        def finish_attn_T():
            for i in range(NOT_):
                u = i % 2
                P.op('dve', lambda e, i=i, u=u: e.scalar_tensor_tensor(out=abf[u][:, :], in0=a2all[:, i, :], scalar=ssall[:, i:i + 1], in1=gd_bc[:, :],
                                                                       op0=ALU.mult, op1=ALU.mult),
                     reads=[f"a2all{i}", f"ssall{i}", "gd_bc"], writes=[f"abf{u}"])
                P.op('pe', lambda e, i=i, u=u: e.transpose(out=pbf[:, i * 128:(i + 1) * 128], in_=abf[u][:, :], identity=ident_bf[:, :]),
                     reads=[f"abf{u}", "ident_bf"], writes=["pbf"])
            P.op('dve', lambda e: e.tensor_copy(out=aT_h[:, :], in_=pbf[:, :]), reads=["pbf"], writes=["aT_h"])

        LA = 1
        NI = len(items)
        for n in range(min(LA, NI)):
            emit_qk(n)
        for n in range(NI):
            if n + LA < NI:
                emit_qk(n + LA)
            emit_exp(n)
            emit_pv(n)
        finish_attn()

        wi = load_w(4096 + h * 128)
        urr = [0]

        def conv_blk(bk, g, dst, d0, first_zero, prev):
            r = urr[0] % 2
            urr[0] += 1
            P.op('act', lambda e: e.copy(out=ub[r][:, 4:516], in_=banks[bk][:, :]), reads=[f"bank{bk}"], writes=[f"ub{r}"])
            if first_zero:
                P.op('dve', lambda e: e.memset(ub[r][:, 0:4], 0.0), writes=[f"ub{r}"])
            elif prev is None:
                P.op('dve', lambda e: e.tensor_copy(out=ub[r][:, 1:4], in_=ub[1 - r][:, 513:516]), reads=[f"ub{1 - r}"], writes=[f"ub{r}"])
            else:
                prev(r)
            P.op('dve', lambda e: e.tensor_scalar(out=uacc[r][:, :], in0=ub[r][:, 1:513], scalar1=cw[:, g, 0:1], scalar2=cb[:, g:g + 1],
                                                  op0=ALU.mult, op1=ALU.add),
                 reads=[f"ub{r}", "cw", "cb"], writes=[f"uacc{r}"])
            for j in range(1, 4):
                P.op('dve', lambda e, j=j: e.scalar_tensor_tensor(out=uacc[r][:, :], in0=ub[r][:, 1 + j:513 + j], scalar=cw[:, g, j:j + 1],
                                                                  in1=uacc[r][:, :], op0=ALU.mult, op1=ALU.add),
                     reads=[f"ub{r}", "cw", f"uacc{r}"], writes=[f"uacc{r}"])
            P.op('act', lambda e: e.activation(out=dst[:, d0:d0 + 512], in_=uacc[r][:, :], func=AF.Silu), reads=[f"uacc{r}"], writes=["kT_d" if dst is kT_m else "qT_d"])

        for blk in range(8):
            proj_fm(wi, blk * 512, 512, lambda bk, blk=blk: conv_blk(bk, 8 + h, kT_m, blk * 512, blk == 0, None))
            if blk == 1:
                finish_attn_T()
                P.op('sp', lambda e, h=h: e.dma_start(out=catT_d[h, :, :], in_=aT_h[:, :]), reads=["aT_h"], writes=["catT_d"], dma=True)
        for g8 in range(4):
            for j in range(8):
                t = g8 * 8 + j
                P.op('pe', lambda e, j=j, t=t: e.transpose(out=pbf[:, j * 128:(j + 1) * 128], in_=kT_m[:, t * 128:(t + 1) * 128],
                                                           identity=ident_bf[:, :]),
                     reads=["kT_d", "ident_bf"], writes=["pbf"])
            P.op('act', lambda e, g8=g8: e.copy(out=k_tok[:, g8 * 8:(g8 + 1) * 8, :], in_=pbf[:, :].rearrange("p (j c) -> p j c", j=8)),
                 reads=["pbf"], writes=["k_tok"])
        P.op('dve', lambda e, h=h: e.tensor_tensor(out=Vp[:, :, 128], in0=beta[:, :].rearrange("p (t c) -> p t c", c=8)[:, :, h],
                                                   in1=valid_s[:, :], op=ALU.mult),
             reads=["beta", "valid", "v_d"], writes=["v_d"])
        wi = load_w(5120 + h * 128)
        P.op('dve', lambda e: e.memset(Z[:, :], 0.0), writes=["Z"])

        def scan_step(c):
            t, half = c // 2, c % 2
            r0 = half * 64
            if c > 0:
                pc = c - 1
                egp = (egA if pc % 2 == 0 else egB)
                col = (pc // 2) * 8 + h
                egap = egp[:, col:col + 1]
            else:
                egap = egA[:, h:h + 1]
            if c >= 48:
                ci = c - 48
                P.op('act', lambda e, ci=ci, egap=egap: e.activation(out=CTb[ci][:, :], in_=Z[:, :], func=AF.Copy, scale=egap),
                     reads=["Z", "egA", "egB"], writes=[f"CTb{ci}"])
            bk = nb()
            P.op('pe', lambda e, bk=bk, t=t, r0=r0: e.matmul(banks[bk][:, 0:129], lhsT=k_tok[r0:r0 + 64, t, :], rhs=Vp[r0:r0 + 64, t, :],
                                                             start=True, stop=True),
                 reads=["k_tok", "v_d"], writes=[f"bank{bk}"])
            P.op('dve', lambda e, bk=bk, egap=egap: e.scalar_tensor_tensor(out=Z[:, :], in0=Z[:, :], scalar=egap, in1=banks[bk][:, 0:129],
                                                                           op0=ALU.mult, op1=ALU.add),
                 reads=["Z", f"bank{bk}", "egA", "egB"], writes=["Z"])

        for g4 in range(8):
            def ev(bk, g4=g4):
                for j in range(4):
                    t = g4 * 4 + j
                    P.op('dve', lambda e, j=j, t=t, bk=bk: e.tensor_scalar(out=Vp[:, t, 0:128], in0=banks[bk][:, j * 128:(j + 1) * 128],
                                                                           scalar1=beta[:, t * 8 + h:t * 8 + h + 1], scalar2=None, op0=ALU.mult),
                         reads=[f"bank{bk}", "beta"], writes=["v_d"])
            proj_tm(wi, g4 * 4, 4, ev)
            if g4 >= 1:
                for c in range(8 * (g4 - 1), 8 * g4):
                    scan_step(c)
        wi = load_w(6144 + h * 128)
        for g4 in range(2):
            proj_tm(wi, T0 + g4 * 4, 4, lambda bk, g4=g4: P.op(
                'act', lambda e: e.activation(out=o_sig[:, g4 * 4:(g4 + 1) * 4, :], in_=banks[bk][:, :].rearrange("p (j c) -> p j c", j=4),
                                              func=AF.Sigmoid),
                reads=[f"bank{bk}"], writes=["o_sig"]))
        for c in range(56, 64):
            scan_step(c)
        wi = load_w(3072 + h * 128)
        qprev = sb_small_qprev
        proj_fm(wi, 3072 - 128, 128, lambda bk: P.op(
            'act', lambda e: e.copy(out=qprev[:, 0:3], in_=banks[bk][:, 125:128]), reads=[f"bank{bk}"], writes=["qprev"]))
        proj_fm(wi, 3072, 512, lambda bk: conv_blk(bk, h, qT_m, 0, False, lambda r: P.op(
            'dve', lambda e: e.tensor_copy(out=ub[r][:, 1:4], in_=qprev[:, 0:3]), reads=["qprev"], writes=[f"ub{r}"])))
        proj_fm(wi, 3072 + 512, 512, lambda bk: conv_blk(bk, h, qT_m, 512, False, None))
        P.op('dve', lambda e: e.memset(qTA[:, :], 0.0), writes=["qTA"])
        P.op('dve', lambda e: e.memset(qTB[:, :], 0.0), writes=["qTB"])
        P.op('dve', lambda e: e.tensor_copy(out=qTA[:, :].rearrange("p (i two l) -> p i two l", two=2, l=64)[:, :, 0, :],
                                            in_=qT_m[:, :].rearrange("p (i two l) -> p i two l", two=2, l=64)[:, :, 0, :]),
             reads=["qT_d", "qTA"], writes=["qTA"])
        P.op('dve', lambda e: e.tensor_copy(out=qTB[:, :].rearrange("p (i two l) -> p i two l", two=2, l=64)[:, :, 1, :],
                                            in_=qT_m[:, :].rearrange("p (i two l) -> p i two l", two=2, l=64)[:, :, 1, :]),
             reads=["qT_d", "qTB"], writes=["qTB"])
        for hf in range(2):
            tiles = list(range(hf * 4, hf * 4 + 4))
            sbk = nb()
            for jj, i in enumerate(tiles):
                t = T0 + i
                P.op('pe', lambda e, sbk=sbk, t=t, i=i, jj=jj: e.matmul(banks[sbk][:, jj * 128:(jj + 1) * 128], lhsT=kT_m[:, t * 128:(t + 1) * 128],
                                                                        rhs=qT_m[:, i * 128:(i + 1) * 128], start=True, stop=True),
                     reads=["kT_d", "qT_d"], writes=[f"bank{sbk}"])
            for jj, i in enumerate(tiles):
                P.op('dve', lambda e, sbk=sbk, jj=jj: e.tensor_tensor(out=SM[jj][:, :], in0=banks[sbk][:, jj * 128:(jj + 1) * 128], in1=cmask[:, :],
                                                                      op=ALU.mult),
                     reads=[f"bank{sbk}", "cmask"], writes=[f"SM{jj}"])
            nbks = [nb(), nb()]
            views = []
            for jj, i in enumerate(tiles):
                t = T0 + i
                nbk = nbks[jj // 2]
                off = (jj % 2) * 129
                views.append((nbk, off))
                P.op('pe', lambda e, nbk=nbk, off=off, jj=jj, t=t: e.matmul(banks[nbk][:, off:off + 129], lhsT=SM[jj][:, :], rhs=Vp[:, t, :],
                                                                            start=True, stop=False),
                     reads=[f"SM{jj}", "v_d"], writes=[f"bank{nbk}"])
                P.op('pe', lambda e, nbk=nbk, off=off, i=i: e.matmul(banks[nbk][:, off:off + 129], lhsT=qTA[:, i * 128:(i + 1) * 128],
                                                                     rhs=CTb[2 * i][:, :], start=False, stop=False),
                     reads=["qTA", f"CTb{2 * i}"], writes=[f"bank{nbk}"])
                P.op('pe', lambda e, nbk=nbk, off=off, i=i: e.matmul(banks[nbk][:, off:off + 129], lhsT=qTB[:, i * 128:(i + 1) * 128],
                                                                     rhs=CTb[2 * i + 1][:, :], start=False, stop=True),
                     reads=["qTB", f"CTb{2 * i + 1}"], writes=[f"bank{nbk}"])

            def den(jj):
                nbk, off = views[jj]
                return banks[nbk][:, off + 128:off + 129]
            for jj, i in enumerate(tiles):
                P.op('dve', lambda e, jj=jj, i=i: e.tensor_scalar(out=smo[:, i, 3:4], in0=den(jj), scalar1=-1.0, scalar2=None, op0=ALU.mult),
                     reads=[f"bank{views[jj][0]}"], writes=[f"smo{i}"])
            for jj, i in enumerate(tiles):
                P.op('dve', lambda e, jj=jj, i=i: e.tensor_tensor(out=smo[:, i, 2:3], in0=den(jj), in1=smo[:, i, 3:4], op=ALU.max),
                     reads=[f"bank{views[jj][0]}", f"smo{i}"], writes=[f"smo{i}"])
            for jj, i in enumerate(tiles):
                col = (T0 + i) * 8 + h
                P.op('dve', lambda e, i=i, col=col: e.tensor_tensor(out=smo[:, i, 2:3], in0=smo[:, i, 2:3], in1=alphac[:, col:col + 1], op=ALU.mult),
                     reads=[f"smo{i}", "alphac"], writes=[f"smo{i}"])
            for jj, i in enumerate(tiles):
                P.op('dve', lambda e, i=i: e.tensor_scalar(out=smo[:, i, 2:3], in0=smo[:, i, 2:3], scalar1=1.0, scalar2=None, op0=ALU.max),
                     reads=[f"smo{i}"], writes=[f"smo{i}"])
            for jj, i in enumerate(tiles):
                P.op('dve', lambda e, i=i: e.reciprocal(out=smo[:, i, 2:3], in_=smo[:, i, 2:3]), reads=[f"smo{i}"], writes=[f"smo{i}"])
            for jj, i in enumerate(tiles):
                col = (T0 + i) * 8 + h
                P.op('dve', lambda e, i=i, col=col: e.tensor_tensor(out=smo[:, i, 2:3], in0=smo[:, i, 2:3], in1=alphac[:, col:col + 1], op=ALU.mult),
                     reads=[f"smo{i}", "alphac"], writes=[f"smo{i}"])
            for jj, i in enumerate(tiles):
                nbk, off = views[jj]
                P.op('dve', lambda e, nbk=nbk, off=off, i=i: e.tensor_scalar(out=a2all[:, i, :], in0=banks[nbk][:, off:off + 128], scalar1=smo[:, i, 2:3],
                                                                             scalar2=None, op0=ALU.mult),
                     reads=[f"bank{nbk}", f"smo{i}"], writes=[f"a2all{i}"])
            for jj, i in enumerate(tiles):
                P.op('dve', lambda e, i=i: e.scalar_tensor_tensor(out=junk[:, :], in0=a2all[:, i, :], scalar=1.0, in1=a2all[:, i, :],
                                                                  op0=ALU.mult, op1=ALU.mult, accum_out=ssall[:, i:i + 1]),
                     reads=[f"a2all{i}", f"ssall{i}"], writes=["junk3", f"ssall{i}"])
        ssk = [f"ssall{i}" for i in range(NOT_)]
        P.op('act', lambda e: e.activation(out=ssall[:, :], in_=ssall[:, :], func=AF.Sqrt, scale=1.0 / 128, bias=eps_t[:, 0:1]),
             reads=ssk + ["eps_t"], writes=ssk)
        P.op('dve', lambda e: e.reciprocal(out=ssall[:, :], in_=ssall[:, :]), reads=ssk, writes=ssk)
        for i in range(NOT_):
            u = i % 2
            P.op('dve', lambda e, i=i, u=u: e.scalar_tensor_tensor(out=a1[u][:, :], in0=a2all[:, i, :], scalar=ssall[:, i:i + 1], in1=gm_bc[:, :],
                                                                   op0=ALU.mult, op1=ALU.mult),
                 reads=[f"a2all{i}", f"ssall{i}", "gm_bc"], writes=[f"a1_{u}"])
            P.op('dve', lambda e, i=i, u=u: e.tensor_tensor(out=abf[u][:, :], in0=a1[u][:, :], in1=o_sig[:, i, :], op=ALU.mult),
                 reads=[f"a1_{u}", "o_sig"], writes=[f"abf{u}"])
            P.op('pe', lambda e, i=i, u=u: e.transpose(out=pbf[:, i * 128:(i + 1) * 128], in_=abf[u][:, :], identity=ident_bf[:, :]),
                 reads=[f"abf{u}", "ident_bf"], writes=["pbf"])
        P.op('dve', lambda e: e.tensor_copy(out=hmT_h[:, :], in_=pbf[:, :]), reads=["pbf"], writes=["aT_h"])
        P.op('sp', lambda e, h=h: e.dma_start(out=catT_d[8 + h, :, :], in_=hmT_h[:, :]), reads=["aT_h"], writes=["catT_d"], dma=True)
        next_kd[0] = load_w(1024 + (h + 1) * 128) if h + 1 < 8 else None
        P.flush()
    P.barrier()
    P.flush()
    if DEBUG:
        P.op('pool', lambda e: e.dma_start(out=dbg["cat"][:, :, :], in_=catT_d[:, :, :]), reads=["catT_d"], dma=True)
        P.op('pool', lambda e: e.dma_start(out=dbg["bias"][:, :], in_=biasd[:, :]), reads=["biasd"], dma=True)
        P.barrier()
        P.flush()


def head_norm_T(P, eps_t, src, srck, sm, smk, junk, g_bc, gk, h, mult, o_sig, oi, tmp, tmpk, obf, obfk, pbf, ident_bf, dstT, dstk, i):
    P.op('dve', lambda e: e.scalar_tensor_tensor(out=junk[:, :], in0=src[:, :], scalar=1.0, in1=src[:, :], op0=ALU.mult, op1=ALU.mult,
                                                 accum_out=sm[:, 4:5]),
         reads=[srck, smk], writes=["junk3", smk])
    P.op('act', lambda e: e.activation(out=sm[:, 4:5], in_=sm[:, 4:5], func=AF.Sqrt, scale=1.0 / 128, bias=eps_t[:, 0:1]),
         reads=[smk, "eps_t"], writes=[smk])
    P.op('dve', lambda e: e.reciprocal(out=sm[:, 4:5], in_=sm[:, 4:5]), reads=[smk], writes=[smk])
    if mult != 1.0:
        P.op('dve', lambda e: e.tensor_scalar(out=sm[:, 4:5], in0=sm[:, 4:5], scalar1=mult, scalar2=None, op0=ALU.mult),
             reads=[smk], writes=[smk])
    if o_sig is None:
        P.op('dve', lambda e: e.scalar_tensor_tensor(out=obf[:, :], in0=src[:, :], scalar=sm[:, 4:5], in1=g_bc[:, :],
                                                     op0=ALU.mult, op1=ALU.mult),
             reads=[srck, smk, gk], writes=[obfk])
    else:
        P.op('dve', lambda e: e.scalar_tensor_tensor(out=tmp[:, :], in0=src[:, :], scalar=sm[:, 4:5], in1=g_bc[:, :],
                                                     op0=ALU.mult, op1=ALU.mult),
             reads=[srck, smk, gk], writes=[tmpk])
        P.op('dve', lambda e: e.tensor_tensor(out=obf[:, :], in0=tmp[:, :], in1=o_sig[:, oi, :], op=ALU.mult),
             reads=[tmpk, "o_sig"], writes=[obfk])
    P.op('pe', lambda e: e.transpose(out=pbf[:, 0:128], in_=obf[:, :], identity=ident_bf[:, :]), reads=[obfk, "ident_bf"], writes=["pbf"])
    P.op('act', lambda e: e.copy(out=dstT[:, i * 128:(i + 1) * 128], in_=pbf[:, 0:128]), reads=["pbf"], writes=[dstk])


def conv_silu(P, upre, uacc, cw, cb, g, n, dst, dstk):
    P.op('dve', lambda e: e.tensor_scalar(out=uacc[:, 0:n], in0=upre[:, 1:1 + n], scalar1=cw[:, g, 0:1], scalar2=cb[:, g:g + 1],
                                          op0=ALU.mult, op1=ALU.add),
         reads=["upre", "cw", "cb"], writes=["uacc"])
    for j in range(1, 4):
        P.op('dve', lambda e, j=j: e.scalar_tensor_tensor(out=uacc[:, 0:n], in0=upre[:, 1 + j:1 + j + n], scalar=cw[:, g, j:j + 1],
                                                          in1=uacc[:, 0:n], op0=ALU.mult, op1=ALU.add),
             reads=["upre", "cw", "uacc"], writes=["uacc"])
    P.op('act', lambda e: e.activation(out=dst[:, 0:n], in_=uacc[:, 0:n], func=AF.Silu), reads=["uacc"], writes=[dstk])


def ffn_phase(nc, P, top, L):
    eps_t = L["eps_t"]
    banks = L["banks"]; pbf = L["pbf"]; ident_f = L["ident_f"]; sb = L["sb"]; dbg = L["dbg"]
    xs = L["xs"]; w_out = L["w_out"]; catT_d = L["catT_d"]; ln_ffn_g = L["ln_ffn_g"]; ln_f_g = L["ln_f_g"]
    w_rt = L["w_rt"]; b_rt = L["b_rt"]; w_gate = L["w_gate"]; w_up = L["w_up"]; w_down = L["w_down"]; y = L["y"]
    brr = [0]

    def nb():
        b = brr[0] % 7
        brr[0] += 1
        return b
    with ExitStack() as es:
        acc = sb(es, "acc", [128, NOT_, D], F32)
        x2T = sb(es, "x2T", [128, 16, OWN], BF16)
        comb = sb(es, "comb", [128, NOT_, 32], F32)
        sm = sb(es, "sm5", [128, 16], F32)
        gmask = sb(es, "gmask", [128, 4], F32)
        ge = sb(es, "ge", [128, 4], F32)
        els = sb(es, "els", [128, 8], F32)
        el2 = sb(es, "el2", [128, 8], F32)
        mk1 = sb(es, "mk1", [128, 8], F32)
        mk2 = sb(es, "mk2", [128, 8], F32)
        ew = sb(es, "ew", [128, 8], F32)
        lg_all = sb(es, "lg_all", [128, NOT_, 36], F32)
        for i in range(NOT_):
            P.op('sp', lambda e, i=i: e.dma_start(out=acc[:, i, :], in_=xs[3072 + i * 128:3072 + (i + 1) * 128, :]), writes=[f"acc{i}"], dma=True)
        with ExitStack() as s4:
            catT = sb(s4, "catT", [128, 16, OWN], BF16)
            P.op('sp', lambda e: e.dma_start(out=catT[:, :, :], in_=catT_d[:, :, :].rearrange("k p t -> p k t")),
                 reads=["catT_d"], writes=["catT"], dma=True)
            wo = [sb(s4, f"wo{i}", [128, 16, 512], BF16) for i in range(2)]
            for n in range(4):
                wi = n % 2
                for q4 in range(4):
                    P.op('pool', lambda e, n=n, wi=wi, q4=q4: e.dma_start(
                        out=wo[wi][:, q4 * 4:(q4 + 1) * 4, :],
                        in_=w_out[q4 * 512:(q4 + 1) * 512, n * 512:(n + 1) * 512].rearrange("(k p) c -> p k c", p=128)),
                        writes=[f"wo{wi}"], dma=True)
                for i in range(NOT_):
                    bk = nb()
                    for kc in range(16):
                        P.op('pe', lambda e, kc=kc, bk=bk, i=i, wi=wi: e.matmul(banks[bk][:, :], lhsT=catT[:, kc, i * 128:(i + 1) * 128],
                                                                               rhs=wo[wi][:, kc, :], start=(kc == 0), stop=(kc == 15)),
                             reads=["catT", f"wo{wi}"], writes=[f"bank{bk}"])
                    P.op('dve', lambda e, bk=bk, i=i, n=n: e.tensor_tensor(out=acc[:, i, n * 512:(n + 1) * 512], in0=banks[bk][:, :],
                                                                           in1=acc[:, i, n * 512:(n + 1) * 512], op=ALU.add),
                         reads=[f"bank{bk}", f"acc{i}"], writes=[f"acc{i}"])
            if DEBUG:
                for i in range(NOT_):
                    P.op('sp', lambda e, i=i: e.dma_start(out=dbg["x1"][i * 128:(i + 1) * 128, :], in_=acc[:, i, :]), reads=[f"acc{i}"], dma=True)
            P.barrier()
            P.flush()
        if STAGES < 5:
            return
        with ExitStack() as s5:
            g2 = sb(s5, "g2_bc", [128, D], F32)
            P.op('sp', lambda e: e.dma_start(out=g2[:, :], in_=bc_ap(ln_ffn_g, D)), writes=["g2"], dma=True)
            wr = sb(s5, "wr", [128, 16, 36], F32)
            for q4 in range(4):
                P.op('sp', lambda e, q4=q4: e.dma_start(out=wr[:, q4 * 4:(q4 + 1) * 4, :],
                                                        in_=w_rt[q4 * 512:(q4 + 1) * 512, :].rearrange("(k p) c -> p k c", p=128)),
                     writes=["wr"], dma=True)
            rb = sb(s5, "rb", [128, 36], F32)
            P.op('sp', lambda e: e.dma_start(out=rb[:, :], in_=bc_ap(b_rt, 36)), writes=["rb"], dma=True)
            x2f = sb(s5, "x2f", [128, D], F32)
            x2T32 = sb(s5, "x2T32", [128, 16, 128], F32)
            junk = sb(s5, "junk5", [128, D], BF16)
            lg = sb(s5, "lg", [128, 36], F32)
            smn = sb(s5, "smn", [128, NOT_], F32)
            x2f2 = [x2f, sb(s5, "x2f_b", [128, D], F32)]
            x2T32_2 = [x2T32, sb(s5, "x2T32_b", [128, 16, 128], F32)]
            for i in range(NOT_):
                P.op('act', lambda e, i=i: e.activation(out=junk[:, :], in_=acc[:, i, :], func=AF.Square, accum_out=smn[:, i:i + 1]),
                     reads=[f"acc{i}"], writes=["junk5", f"smn{i}"])
            smk = [f"smn{i}" for i in range(NOT_)]
            P.op('act', lambda e: e.activation(out=smn[:, :], in_=smn[:, :], func=AF.Sqrt, scale=1.0 / D, bias=eps_t[:, 0:1]),
                 reads=smk + ["eps_t"], writes=smk)
            P.op('dve', lambda e: e.reciprocal(out=smn[:, :], in_=smn[:, :]), reads=smk, writes=smk)
            for i in range(NOT_):
                ub_ = i % 2
                xf = x2f2[ub_]
                xt32 = x2T32_2[ub_]
                P.op('dve', lambda e, i=i, xf=xf: e.scalar_tensor_tensor(out=xf[:, :], in0=acc[:, i, :], scalar=smn[:, i:i + 1], in1=g2[:, :],
                                                                         op0=ALU.mult, op1=ALU.mult),
                     reads=[f"acc{i}", f"smn{i}", "g2"], writes=[f"x2f{ub_}"])
                for q4 in range(4):
                    bk = nb()
                    for j in range(4):
                        kc = q4 * 4 + j
                        P.op('pe', lambda e, bk=bk, j=j, kc=kc, xf=xf: e.transpose(out=banks[bk][:, j * 128:(j + 1) * 128], in_=xf[:, kc * 128:(kc + 1) * 128],
                                                                                   identity=ident_f[:, :]),
                             reads=[f"x2f{ub_}", "ident_f"], writes=[f"bank{bk}"])
                    P.op('act', lambda e, bk=bk, q4=q4, xt32=xt32: e.copy(out=xt32[:, q4 * 4:(q4 + 1) * 4, :], in_=banks[bk][:, :].rearrange("p (j c) -> p j c", j=4)),
                         reads=[f"bank{bk}"], writes=[f"x2T32{ub_}"])
                    P.op('dve', lambda e, q4=q4, i=i, xt32=xt32: e.tensor_copy(out=x2T[:, q4 * 4:(q4 + 1) * 4, i * 128:(i + 1) * 128],
                                                                               in_=xt32[:, q4 * 4:(q4 + 1) * 4, :]),
                         reads=[f"x2T32{ub_}"], writes=["x2T"])
                bk = nb()
                for kc in range(16):
                    P.op('pe', lambda e, bk=bk, kc=kc, xt32=xt32: e.matmul(banks[bk][:, 0:36], lhsT=xt32[:, kc, :], rhs=wr[:, kc, :], start=(kc == 0), stop=(kc == 15)),
                         reads=[f"x2T32{ub_}", "wr"], writes=[f"bank{bk}"])
                P.op('dve', lambda e, bk=bk, i=i: e.tensor_tensor(out=lg_all[:, i, :], in0=banks[bk][:, 0:36], in1=rb[:, :], op=ALU.add),
                     reads=[f"bank{bk}", "rb"], writes=[f"lg{i}"])
            P.barrier()
            P.flush()

        def route_tile(i):
            if True:
                R = ["gmask", "ge", "els", "el2", "mk1", "mk2", "ew", "sm5", f"lg{i}"]
                lg = lg_all[:, i, :]
                P.op('dve', lambda e, lg=lg: e.tensor_reduce(out=sm[:, 1:2], in_=lg[:, 0:4], axis=AX.X, op=ALU.max), reads=R, writes=["sm5"])
                P.op('dve', lambda e, lg=lg: e.tensor_scalar(out=gmask[:, :], in0=lg[:, 0:4], scalar1=sm[:, 1:2], scalar2=None, op0=ALU.is_equal),
                     reads=R, writes=["gmask"])
                P.op('dve', lambda e: e.tensor_scalar(out=sm[:, 2:3], in0=sm[:, 1:2], scalar1=-1.0, scalar2=None, op0=ALU.mult), reads=R, writes=["sm5"])
                P.op('act', lambda e, lg=lg: e.activation(out=ge[:, :], in_=lg[:, 0:4], func=AF.Exp, bias=sm[:, 2:3], scale=1.0, accum_out=sm[:, 3:4]),
                     reads=R, writes=["ge", "sm5"])
                P.op('dve', lambda e: e.reciprocal(out=sm[:, 3:4], in_=sm[:, 3:4]), reads=R, writes=["sm5"])
                P.op('dve', lambda e, lg=lg: e.tensor_scalar(out=els[:, :], in0=lg[:, 4:12], scalar1=gmask[:, 0:1], scalar2=None, op0=ALU.mult), reads=R, writes=["els"])
                for g in range(1, 4):
                    P.op('dve', lambda e, g=g, lg=lg: e.scalar_tensor_tensor(out=els[:, :], in0=lg[:, 4 + 8 * g:12 + 8 * g], scalar=gmask[:, g:g + 1],
                                                                      in1=els[:, :], op0=ALU.mult, op1=ALU.add), reads=R, writes=["els"])
                P.op('dve', lambda e: e.tensor_reduce(out=sm[:, 4:5], in_=els[:, :], axis=AX.X, op=ALU.max), reads=R, writes=["sm5"])
                P.op('dve', lambda e: e.tensor_scalar(out=mk1[:, :], in0=els[:, :], scalar1=sm[:, 4:5], scalar2=None, op0=ALU.is_equal), reads=R, writes=["mk1"])
                P.op('dve', lambda e: e.scalar_tensor_tensor(out=el2[:, :], in0=mk1[:, :], scalar=-1e30, in1=els[:, :], op0=ALU.mult, op1=ALU.add),
                     reads=R, writes=["el2"])
                P.op('dve', lambda e: e.tensor_reduce(out=sm[:, 5:6], in_=el2[:, :], axis=AX.X, op=ALU.max), reads=R, writes=["sm5"])
                P.op('dve', lambda e: e.tensor_scalar(out=mk2[:, :], in0=el2[:, :], scalar1=sm[:, 5:6], scalar2=None, op0=ALU.is_equal), reads=R, writes=["mk2"])
                P.op('dve', lambda e: e.tensor_tensor(out=sm[:, 6:7], in0=sm[:, 5:6], in1=sm[:, 4:5], op=ALU.subtract), reads=R, writes=["sm5"])
                P.op('act', lambda e: e.activation(out=sm[:, 6:7], in_=sm[:, 6:7], func=AF.Exp), reads=R, writes=["sm5"])
                P.op('dve', lambda e: e.tensor_scalar(out=sm[:, 7:8], in0=sm[:, 6:7], scalar1=1.0, scalar2=None, op0=ALU.add), reads=R, writes=["sm5"])
                P.op('dve', lambda e: e.reciprocal(out=sm[:, 7:8], in_=sm[:, 7:8]), reads=R, writes=["sm5"])
                P.op('dve', lambda e: e.tensor_tensor(out=sm[:, 8:9], in0=sm[:, 6:7], in1=sm[:, 7:8], op=ALU.mult), reads=R, writes=["sm5"])
                P.op('dve', lambda e: e.tensor_tensor(out=sm[:, 7:8], in0=sm[:, 7:8], in1=sm[:, 3:4], op=ALU.mult), reads=R, writes=["sm5"])
                P.op('dve', lambda e: e.tensor_tensor(out=sm[:, 8:9], in0=sm[:, 8:9], in1=sm[:, 3:4], op=ALU.mult), reads=R, writes=["sm5"])
                P.op('dve', lambda e: e.tensor_scalar(out=ew[:, :], in0=mk1[:, :], scalar1=sm[:, 7:8], scalar2=None, op0=ALU.mult), reads=R, writes=["ew"])
                P.op('dve', lambda e: e.scalar_tensor_tensor(out=ew[:, :], in0=mk2[:, :], scalar=sm[:, 8:9], in1=ew[:, :], op0=ALU.mult, op1=ALU.add),
                     reads=R, writes=["ew"])
                for g in range(4):
                    P.op('dve', lambda e, g=g, i=i: e.tensor_scalar(out=comb[:, i, g * 8:(g + 1) * 8], in0=ew[:, :], scalar1=gmask[:, g:g + 1], scalar2=None,
                                                                    op0=ALU.mult), reads=R, writes=[f"comb{i}"])
        if STAGES < 6:
            return
        with ExitStack() as s6:
            NSLOT = 5
            slots = [sb(s6, f"wsl{i}", [128, 8192], BF16) for i in range(NSLOT)]
            hid = [sb(s6, f"hid{i}", [128, 4, OWN], BF16) for i in range(1)]
            sg = [sb(s6, f"sg{i}", [128, 512], BF16) for i in range(2)]
            print("SBUF remaining in experts:", nc.sbuf_bytes_remaining)
            def wviews(ex):
                sl = [(3 * ex + j) % NSLOT for j in range(3)]
                wgv = slots[sl[0]][:, :].rearrange("p (k f) -> p k f", k=16)
                wuv = slots[sl[1]][:, :].rearrange("p (k f) -> p k f", k=16)
                wdv = slots[sl[2]][:, :].rearrange("p (k f) -> p k f", k=4)
                return wgv, wuv, wdv, f"wsl{sl[0]}", f"wsl{sl[1]}", f"wsl{sl[2]}"

            def load_expert(ex):
                wgv, wuv, wdv, wgk, wuk, wdk = wviews(ex)
                for q4 in range(4):
                    P.op('pool', lambda e, ex=ex, q4=q4, wgv=wgv: e.dma_start(
                        out=wgv[:, q4 * 4:(q4 + 1) * 4, :], in_=w_gate[ex, q4 * 512:(q4 + 1) * 512, :].rearrange("(k p) f -> p k f", p=128)),
                        writes=[wgk], dma=True)
                for q4 in range(4):
                    P.op('pool', lambda e, ex=ex, q4=q4, wuv=wuv: e.dma_start(
                        out=wuv[:, q4 * 4:(q4 + 1) * 4, :], in_=w_up[ex, q4 * 512:(q4 + 1) * 512, :].rearrange("(k p) f -> p k f", p=128)),
                        writes=[wuk], dma=True)
                for q4 in range(4):
                    P.op('pool', lambda e, ex=ex, q4=q4, wdv=wdv: e.dma_start(
                        out=wdv[:, q4:q4 + 1, :], in_=w_down[ex, q4 * 128:(q4 + 1) * 128, :].rearrange("(k p) f -> p k f", p=128)),
                        writes=[wdk], dma=True)

            load_expert(0)
            for ex in range(32):
                wgv, wuv, wdv, wgk, wuk, wdk = wviews(ex)
                for fc in range(4):
                    for th in range(2):
                        gb_ = nb()
                        for kc in range(16):
                            P.op('pe', lambda e, gb_=gb_, kc=kc, fc=fc, th=th, wgv=wgv: e.matmul(banks[gb_][:, :], lhsT=wgv[:, kc, fc * 128:(fc + 1) * 128],
                                                                                            rhs=x2T[:, kc, th * 512:(th + 1) * 512], start=(kc == 0), stop=(kc == 15)),
                                 reads=[wgk, "x2T"], writes=[f"bank{gb_}"])
                        ub_ = nb()
                        for kc in range(16):
                            P.op('pe', lambda e, ub_=ub_, kc=kc, fc=fc, th=th, wuv=wuv: e.matmul(banks[ub_][:, :], lhsT=wuv[:, kc, fc * 128:(fc + 1) * 128],
                                                                                            rhs=x2T[:, kc, th * 512:(th + 1) * 512], start=(kc == 0), stop=(kc == 15)),
                                 reads=[wuk, "x2T"], writes=[f"bank{ub_}"])
                        si = (fc * 2 + th) % 2
                        P.op('act', lambda e, gb_=gb_, si=si: e.activation(out=sg[si][:, :], in_=banks[gb_][:, :], func=AF.Silu),
                             reads=[f"bank{gb_}"], writes=[f"sg{si}"])
                        P.op('dve', lambda e, ub_=ub_, si=si, fc=fc, th=th: e.tensor_tensor(out=hid[0][:, fc, th * 512:(th + 1) * 512], in0=banks[ub_][:, :],
                                                                                           in1=sg[si][:, :], op=ALU.mult),
                             reads=[f"bank{ub_}", f"sg{si}"], writes=["hid"])
                        if ex == 0:
                            route_tile(fc * 2 + th)
                if ex + 1 < 32:
                    load_expert(ex + 1)
                for i in range(NOT_):
                    for n in range(4):
                        bk = nb()
                        for fc in range(4):
                            P.op('pe', lambda e, bk=bk, fc=fc, i=i, n=n, wdv=wdv: e.matmul(banks[bk][:, :], lhsT=hid[0][:, fc, i * 128:(i + 1) * 128],
                                                                                      rhs=wdv[:, fc, n * 512:(n + 1) * 512], start=(fc == 0), stop=(fc == 3)),
                                 reads=["hid", wdk], writes=[f"bank{bk}"])
                        P.op('dve', lambda e, bk=bk, i=i, n=n, ex=ex: e.scalar_tensor_tensor(out=acc[:, i, n * 512:(n + 1) * 512], in0=banks[bk][:, :],
                                                                                            scalar=comb[:, i, ex:ex + 1], in1=acc[:, i, n * 512:(n + 1) * 512],
                                                                                            op0=ALU.mult, op1=ALU.add),
                             reads=[f"bank{bk}", f"comb{i}", f"acc{i}"], writes=[f"acc{i}"])
                if ex % 8 == 7:
                    P.flush()
            P.barrier()
            P.flush()
        if STAGES < 7:
            return
        with ExitStack() as s7:
            g3 = sb(s7, "g3_bc", [128, D], F32)
            P.op('sp', lambda e: e.dma_start(out=g3[:, :], in_=bc_ap(ln_f_g, D)), writes=["g3"], dma=True)
            junk = sb(s7, "junk7", [128, D], BF16)
            sm = sb(s7, "sm7", [128, NOT_], F32)
            ob = [sb(s7, f"ob{i}", [128, D], F32) for i in range(2)]
            for i in range(NOT_):
                u = i % 2
                P.op('act', lambda e, i=i: e.activation(out=junk[:, :], in_=acc[:, i, :], func=AF.Square, accum_out=sm[:, i:i + 1]),
                     reads=[f"acc{i}"], writes=["junk7", f"sm7_{i}"])
                P.op('act', lambda e, i=i: e.activation(out=sm[:, i:i + 1], in_=sm[:, i:i + 1], func=AF.Sqrt, scale=1.0 / D, bias=eps_t[:, 0:1]),
                     reads=[f"sm7_{i}", "eps_t"], writes=[f"sm7_{i}"])
                P.op('dve', lambda e, i=i: e.reciprocal(out=sm[:, i:i + 1], in_=sm[:, i:i + 1]), reads=[f"sm7_{i}"], writes=[f"sm7_{i}"])
                P.op('dve', lambda e, i=i, u=u: e.scalar_tensor_tensor(out=ob[u][:, :], in0=acc[:, i, :], scalar=sm[:, i:i + 1], in1=g3[:, :],
                                                                       op0=ALU.mult, op1=ALU.mult),
                     reads=[f"acc{i}", f"sm7_{i}", "g3"], writes=[f"ob{u}"])
                P.op('sp', lambda e, i=i, u=u: e.dma_start(out=y[i * 128:(i + 1) * 128, :], in_=ob[u][:, :]), reads=[f"ob{u}"], writes=["y"], dma=True)
            P.final_wait('sp')
            P.flush()


_CACHE = {}


def _consts():
    if "c" in _CACHE:
        return _CACHE["c"]
    bt = _bucket_table()
    E = np.zeros((33, 2, 128, 128), np.float32)
    k = np.arange(128)[:, None]
    q = np.arange(128)[None, :]
    for di, d in enumerate((-1, 0)):
        rel = 128 * d + k - q
        bk = bt[rel + 255]
        for b in range(32):
            E[b, di] = (bk == b)
        if d == 0:
            E[32, di] = ((k // 64) > (q // 64))
    tri = np.zeros((128, 3, 128), np.float32)
    lp = np.arange(128)[:, None]
    l = np.arange(128)[None, :]
    tri[:, 0, :] = ((lp // 64) == (l // 64)) & (lp <= l)
    tri[:, 1, :] = (lp < 64) * np.ones((1, 128))
    tri[:, 2, :] = (lp >= 64) * np.ones((1, 128))
    cmask = (((lp // 64) == (l // 64)) & (lp <= l)).astype(np.float32)
    c = dict(Econst=E.reshape(33, -1), ident_bf=np.eye(128, dtype=np.float32).astype(ml_dtypes.bfloat16),
             ident_f=np.eye(128, dtype=np.float32), tri=tri.reshape(128, 384), cmask=cmask)
    _CACHE["c"] = c
    return c


def make_in_maps(x, rel_bias, ln_mix_g, w_in, conv_w, conv_b, b_i, b_f, lam_q1, lam_k1, lam_q2, lam_k2, diff_norm_g,
                 mlstm_norm_g, w_out, ln_ffn_g, w_group, b_group, w_router, b_router, w_gate, w_up, w_down, ln_f_g):
    f = lambda a: np.ascontiguousarray(np.asarray(a, dtype=np.float32))
    x = f(x)
    c = _consts()
    w_rt = np.concatenate([f(w_group)[0], f(w_router)[0].transpose(1, 0, 2).reshape(D, 32)], axis=1)
    b_rt = np.concatenate([f(b_group)[0], f(b_router)[0].reshape(32)])
    relb = np.concatenate([f(rel_bias), np.full((1, 8), -30000.0, np.float32)], axis=0)
    shared = dict(
        w_in=f(w_in)[0], w_out=f(w_out)[0], w_gate=f(w_gate)[0].reshape(32, D, 512), w_up=f(w_up)[0].reshape(32, D, 512),
        w_down=f(w_down)[0].reshape(32, 512, D), w_rt=np.ascontiguousarray(w_rt), b_rt=b_rt,
        ln_mix_g=f(ln_mix_g)[0], ln_ffn_g=f(ln_ffn_g)[0], ln_f_g=f(ln_f_g),
        conv_wt=np.ascontiguousarray(f(conv_w)[0].T.reshape(16, 128, 4).transpose(1, 0, 2).reshape(128, 64)),
        conv_b=np.ascontiguousarray(f(conv_b)[0].reshape(16, 128).T), b_if=np.concatenate([f(b_i)[0], f(b_f)[0]]),
        lam_v=np.stack([f(lam_q1)[0], f(lam_k1)[0], f(lam_q2)[0], f(lam_k2)[0]]),
        diff_norm_g=f(diff_norm_g)[0], mlstm_norm_g=f(mlstm_norm_g)[0], relb=relb, **c)
    maps = []
    for core in range(8):
        b, j = core // 4, core % 4
        pad = 3072 - 1024 * j
        xsl = np.zeros((S, D), np.float32)
        xsl[pad:] = x[b, :1024 * (j + 1)]
        v = np.zeros(S, np.float32)
        v[pad:] = 1.0
        m = dict(shared)
        m["xs"] = xsl
        m["valid"] = np.ascontiguousarray(v.reshape(NT, 128).T)
        maps.append(m)
    return maps


def kernel(**inputs):
    if "nc" not in _CACHE:
        _CACHE["nc"] = build_program()
    nc = _CACHE["nc"]
    maps = make_in_maps(**inputs)
    res = run_bass_kernel_spmd(nc, maps, core_ids=list(range(8)))
    _CACHE["res"] = res
    out = np.zeros((2, S, D), np.float32)
    for core in range(8):
        b, j = core // 4, core % 4
        out[b, j * 1024:(j + 1) * 1024] = np.asarray(res.results[core]["y"])
    return out
```
